# Optimizing a Trainium2 kernel written in Bass

```python
import math
import jax
import jax.numpy as jnp
from jax import lax
import numpy as np

D_MODEL = 4096
BATCH = 4
SEQ = 2048
DEPTH = 2

MEM_LEN = 256
HEAD_DIM = 128
BW = D_MODEL // 4
N_BRANCH = 4
A_HEADS = BW // HEAD_DIM
MOBA_BLOCK = 256
MOBA_TOPK = 3
MOBA_Q_BLOCK = 32
B_HEADS = BW // (2 * HEAD_DIM)
B_V_DIM = 2 * HEAD_DIM
ATTN_Q_BLOCK = 128
SGU_CHUNK = 128
SGU_GROUP_CH = 128
SGU_GROUPS = BW // SGU_GROUP_CH
M_HEADS = 4
M_HEAD_DIM = BW // M_HEADS
N_ALIBI_HEADS = A_HEADS + B_HEADS
RMS_EPS = 1e-6
LN_EPS = 1e-5
NEG = -1e30
W_IN_COLS = 13 * BW + N_BRANCH * D_MODEL
SPLIT_POINTS = tuple(BW * i for i in range(1, 14))

kernel_name = "hybrid_moba_diff_sgu_mem_gated"


def rmsnorm(x, g, eps=RMS_EPS):
    xf = x.astype(jnp.float32)
    y = xf * lax.rsqrt(jnp.mean(xf * xf, axis=-1, keepdims=True) + eps)
    return (y * g.astype(jnp.float32)).astype(x.dtype)


def layernorm(x, g, b, eps=LN_EPS):
    xf = x.astype(jnp.float32)
    mu = jnp.mean(xf, axis=-1, keepdims=True)
    var = jnp.mean(jnp.square(xf - mu), axis=-1, keepdims=True)
    y = (xf - mu) * lax.rsqrt(var + eps)
    return (y * g.astype(jnp.float32) + b.astype(jnp.float32)).astype(x.dtype)


def alibi_slopes(n):
    return jnp.exp2(-8.0 * jnp.arange(1, n + 1, dtype=jnp.float32) / n)


def moba_attention(q, k, v, slopes):
    bsz, seq, nh, hd = q.shape
    nb = -(-seq // MOBA_BLOCK)
    pad = nb * MOBA_BLOCK - seq
    padw = ((0, 0), (0, pad), (0, 0), (0, 0))
    kb = jnp.pad(k, padw).reshape(bsz, nb, MOBA_BLOCK, nh, hd).transpose(0, 3, 1, 2, 4)
    vb = jnp.pad(v, padw).reshape(bsz, nb, MOBA_BLOCK, nh, hd).transpose(0, 3, 1, 2, 4)
    k_mean = jnp.mean(kb, axis=3)
    q_blk = jnp.arange(seq) // MOBA_BLOCK
    gate = jnp.einsum('bshd,bhnd->bhsn', q, k_mean).astype(jnp.float32)
    past = jnp.arange(nb)[None, :] < q_blk[:, None]
    gate = jnp.where(past, gate, NEG)
    kk = max(min(MOBA_TOPK, nb - 1), 1)
    _, sel = lax.top_k(gate, kk)
    valid = sel < q_blk[None, None, :, None]

    nq = seq // MOBA_Q_BLOCK
    q_c = q.reshape(bsz, nq, MOBA_Q_BLOCK, nh, hd).transpose(1, 0, 2, 3, 4)
    sel_c = sel.reshape(bsz, nh, nq, MOBA_Q_BLOCK, kk).transpose(2, 0, 1, 3, 4)
    val_c = valid.reshape(bsz, nh, nq, MOBA_Q_BLOCK, kk).transpose(2, 0, 1, 3, 4)
    bi = jnp.arange(bsz)[:, None, None, None]
    hi = jnp.arange(nh)[None, :, None, None]
    offs = jnp.arange(MOBA_BLOCK)
    scale = hd ** -0.5

    def step(args):
        qi, qc, sc_idx, vmask = args
        t = qi * MOBA_Q_BLOCK + jnp.arange(MOBA_Q_BLOCK)
        own = (qi * MOBA_Q_BLOCK) // MOBA_BLOCK
        k_own = lax.dynamic_index_in_dim(kb, own, axis=2, keepdims=False)
        v_own = lax.dynamic_index_in_dim(vb, own, axis=2, keepdims=False)
        d_own = t[:, None] - (own * MOBA_BLOCK + offs)[None, :]
        s_own = (jnp.einsum('bqhd,bhpd->bhqp', qc, k_own).astype(jnp.float32) * scale
                 - slopes[None, :, None, None] * jnp.abs(d_own).astype(jnp.float32))
        s_own = jnp.where(d_own >= 0, s_own, NEG)
        k_sel = kb[bi, hi, sc_idx]
        v_sel = vb[bi, hi, sc_idx]
        d_sel = t[None, None, :, None, None] - (sc_idx[..., None] * MOBA_BLOCK + offs)
        s_sel = (jnp.einsum('bqhd,bhqjpd->bhqjp', qc, k_sel).astype(jnp.float32) * scale
                 - slopes[None, :, None, None, None] * jnp.abs(d_sel).astype(jnp.float32))
        s_sel = jnp.where(vmask[..., None], s_sel, NEG)
        s_all = jnp.concatenate([s_own, s_sel.reshape(bsz, nh, MOBA_Q_BLOCK, kk * MOBA_BLOCK)], axis=-1)
        p = jax.nn.softmax(s_all, axis=-1).astype(v.dtype)
        p_own = p[..., :MOBA_BLOCK]
        p_sel = p[..., MOBA_BLOCK:].reshape(bsz, nh, MOBA_Q_BLOCK, kk, MOBA_BLOCK)
        return (jnp.einsum('bhqp,bhpd->bqhd', p_own, v_own)
                + jnp.einsum('bhqjp,bhqjpd->bqhd', p_sel, v_sel))

    out = lax.map(step, (jnp.arange(nq), q_c, sel_c, val_c))
    return out.transpose(1, 0, 2, 3, 4).reshape(bsz, seq, nh, hd)


def diff_attention(q, k, v, lam, slopes):
    bsz, seq, nh, _, dq = q.shape
    dv = v.shape[-1]
    nq = seq // ATTN_Q_BLOCK
    q_c = q.reshape(bsz, nq, ATTN_Q_BLOCK, nh, 2, dq).transpose(1, 0, 2, 3, 4, 5)
    s_pos = jnp.arange(seq)
    scale = dq ** -0.5

    def step(args):
        qi, qc = args
        t = qi * ATTN_Q_BLOCK + jnp.arange(ATTN_Q_BLOCK)
        d = t[:, None] - s_pos[None, :]
        sc = (jnp.einsum('bqhmd,bshmd->bhmqs', qc, k).astype(jnp.float32) * scale
              - slopes[None, :, None, None, None] * jnp.abs(d).astype(jnp.float32))
        sc = jnp.where(d >= 0, sc, NEG)
        p = jax.nn.softmax(sc, axis=-1)
        w = (p[:, :, 0] - lam * p[:, :, 1]).astype(v.dtype)
        return jnp.einsum('bhqs,bshd->bqhd', w, v)

    out = lax.map(step, (jnp.arange(nq), q_c))
    return out.transpose(1, 0, 2, 3, 4).reshape(bsz, seq, nh, dv)


def spatial_gating(u, v, ln_g, ln_b, w_s, b_s):
    bsz, seq, c = u.shape
    nc = seq // SGU_CHUNK
    vn = layernorm(v, ln_g, ln_b).reshape(bsz, nc, SGU_CHUNK, SGU_GROUPS, SGU_GROUP_CH)
    causal = jnp.tril(jnp.ones((SGU_CHUNK, SGU_CHUNK), dtype=bool))
    w = jnp.where(causal[None], w_s, jnp.zeros_like(w_s))
    mixed = jnp.einsum('gts,bcsge->bctge', w, vn) + b_s.T[None, None, :, :, None]
    return u * mixed.reshape(bsz, seq, c)


def memory_attention(q, mem_k, mem_v):
    scale = q.shape[-1] ** -0.5
    sc = jnp.einsum('bshd,bmhd->bhsm', q, mem_k).astype(jnp.float32) * scale
    p = jax.nn.softmax(sc, axis=-1).astype(mem_v.dtype)
    return jnp.einsum('bhsm,bmhd->bshd', p, mem_v)


def hybrid_layer(x, mem, layer_idx, norm_g, w_in, mem_norm_g, w_mem_kv, lam_q1, lam_k1, lam_q2, lam_k2,
                 diff_subln_g, sgu_ln_g, sgu_ln_b, sgu_w, sgu_b, w_branch, w_out):
    bsz, seq, _ = x.shape
    h = rmsnorm(x, norm_g)
    proj = h @ w_in
    (qa, ka, va, za, qb, kb, vb, zb, uc, vc, zc, qm, zm, gate_logits) = jnp.split(proj, SPLIT_POINTS, axis=-1)
    slopes = alibi_slopes(N_ALIBI_HEADS)

    sa = (bsz, seq, A_HEADS, HEAD_DIM)
    ya = moba_attention(qa.reshape(sa), ka.reshape(sa), va.reshape(sa), slopes[B_HEADS:])
    ya = ya.reshape(bsz, seq, BW) * jax.nn.silu(za)

    lam_init = 0.8 - 0.6 * math.exp(-0.3 * layer_idx)
    lam = (jnp.exp(jnp.sum(lam_q1.astype(jnp.float32) * lam_k1.astype(jnp.float32)))
           - jnp.exp(jnp.sum(lam_q2.astype(jnp.float32) * lam_k2.astype(jnp.float32))) + lam_init)
    sb = (bsz, seq, B_HEADS, 2, HEAD_DIM)
    ob = diff_attention(qb.reshape(sb), kb.reshape(sb), vb.reshape(bsz, seq, B_HEADS, B_V_DIM), lam,
                        slopes[:B_HEADS])
    ob = rmsnorm(ob, diff_subln_g) * (1.0 - lam_init)
    yb = ob.reshape(bsz, seq, BW) * jax.nn.silu(zb)

    yc = spatial_gating(jax.nn.gelu(uc), jax.nn.gelu(vc), sgu_ln_g, sgu_ln_b, sgu_w, sgu_b) * jax.nn.silu(zc)

    mlen = mem.shape[1]
    mk, mv = jnp.split(rmsnorm(mem, mem_norm_g) @ w_mem_kv, 2, axis=-1)
    sm = (bsz, mlen, M_HEADS, M_HEAD_DIM)
    ym = memory_attention(qm.reshape(bsz, seq, M_HEADS, M_HEAD_DIM), mk.reshape(sm), mv.reshape(sm))
    ym = ym.reshape(bsz, seq, BW) * jax.nn.silu(zm)

    gates = jax.nn.sigmoid(gate_logits.reshape(bsz, seq, N_BRANCH, D_MODEL))
    merged = gates[:, :, 0] * (ya @ w_branch[0])
    merged = merged + gates[:, :, 1] * (yb @ w_branch[1])
    merged = merged + gates[:, :, 2] * (yc @ w_branch[2])
    merged = merged + gates[:, :, 3] * (ym @ w_branch[3])
    return x + merged @ w_out


def setup_inputs(seed: int = 0) -> dict:
    key = jax.random.key(seed)
    ks = jax.random.split(key, 20)
    f32 = jnp.float32
    nrm = lambda k, shape, s: jax.random.normal(k, shape, f32) * s
    return {
        "x": nrm(ks[0], (BATCH, SEQ, D_MODEL), 1.0),
        "mem": nrm(ks[1], (BATCH, MEM_LEN, D_MODEL), 1.0),
        "norm_g": 1.0 + nrm(ks[2], (DEPTH, D_MODEL), 0.02),
        "w_in": nrm(ks[3], (DEPTH, D_MODEL, W_IN_COLS), D_MODEL ** -0.5),
        "mem_norm_g": 1.0 + nrm(ks[4], (DEPTH, D_MODEL), 0.02),
        "w_mem_kv": nrm(ks[5], (DEPTH, D_MODEL, 2 * BW), D_MODEL ** -0.5),
        "diff_lam_q1": nrm(ks[6], (DEPTH, HEAD_DIM), 0.1),
        "diff_lam_k1": nrm(ks[7], (DEPTH, HEAD_DIM), 0.1),
        "diff_lam_q2": nrm(ks[8], (DEPTH, HEAD_DIM), 0.1),
        "diff_lam_k2": nrm(ks[9], (DEPTH, HEAD_DIM), 0.1),
        "diff_subln_g": 1.0 + nrm(ks[10], (DEPTH, B_V_DIM), 0.02),
        "sgu_ln_g": 1.0 + nrm(ks[11], (DEPTH, BW), 0.02),
        "sgu_ln_b": nrm(ks[12], (DEPTH, BW), 0.02),
        "sgu_w": nrm(ks[13], (DEPTH, SGU_GROUPS, SGU_CHUNK, SGU_CHUNK), SGU_CHUNK ** -0.5),
        "sgu_b": 1.0 + nrm(ks[14], (DEPTH, SGU_GROUPS, SGU_CHUNK), 0.02),
        "w_branch": nrm(ks[15], (DEPTH, N_BRANCH, BW, D_MODEL), BW ** -0.5),
        "w_out": nrm(ks[16], (DEPTH, D_MODEL, D_MODEL), D_MODEL ** -0.5),
        "final_g": 1.0 + nrm(ks[17], (D_MODEL,), 0.02),
    }


def reference(x, mem, norm_g, w_in, mem_norm_g, w_mem_kv, diff_lam_q1, diff_lam_k1, diff_lam_q2, diff_lam_k2,
              diff_subln_g, sgu_ln_g, sgu_ln_b, sgu_w, sgu_b, w_branch, w_out, final_g):
    for l in range(DEPTH):
        x = hybrid_layer(x, mem, l, norm_g[l], w_in[l], mem_norm_g[l], w_mem_kv[l],
                         diff_lam_q1[l], diff_lam_k1[l], diff_lam_q2[l], diff_lam_k2[l],
                         diff_subln_g[l], sgu_ln_g[l], sgu_ln_b[l], sgu_w[l], sgu_b[l],
                         w_branch[l], w_out[l])
    return rmsnorm(x, final_g)
```

```python
import math
import numpy as np
import concourse.bass as bass
import concourse.mybir as mybir
from concourse.bass_utils import run_bass_kernel_spmd

F32 = mybir.dt.float32
BF16 = mybir.dt.bfloat16
AF = mybir.ActivationFunctionType
ALU = mybir.AluOpType
AX = mybir.AxisListType

RMS_EPS = 1e-6
LN_EPS = 1e-5
NEGB = -30000.0


class Cfg:
    def __init__(self, D=4096, depth=2):
        self.D = D
        self.depth = depth
        self.S = 2048
        self.T = 1024
        self.KC = D // 128
        self.BW = D // 4
        self.BC = self.BW // 128
        self.AH = self.BW // 128
        self.BH = self.BW // 256
        self.SG = self.BW // 128
        self.MH = self.BW // 256
        self.NAL = self.AH + self.BH
        self.WIN = 13 * self.BW + 4 * D
        self.ML = 256
        self.ncores = 8


class Tok:
    __slots__ = ("w", "r", "dsem", "dcnt", "name")

    def __init__(self, name=""):
        self.w = {}
        self.r = {}
        self.dsem = None
        self.dcnt = 0
        self.name = name


class Sched:
    def __init__(self, nc):
        self.nc = nc
        self.engs = {"pe": nc.tensor, "act": nc.scalar, "dve": nc.vector, "pool": nc.gpsimd, "sp": nc.sync}
        self.sem = {}
        self.cnt = {}
        self.seen = {k: {} for k in self.engs}
        self.nsem = 0
        self.all_sems = {}
        self.dtoks = []
        self.free_dsems = []
        for k in self.engs:
            self.new_engine_sem(k)

    def alloc_sem(self, name):
        s = self.nc.alloc_semaphore(name=f"{name}_{self.nsem}")
        self.nsem += 1
        self.all_sems[id(s)] = [s, 0]
        return s

    def new_engine_sem(self, k):
        self.sem[k] = self.alloc_sem("e" + k)
        self.cnt[k] = 0

    def _note(self, sem, val):
        self.all_sems[id(sem)][1] = max(self.all_sems[id(sem)][1], val)

    def _wait(self, ename, deps):
        need = {}
        for d in deps:
            for key, (s, v) in d.items():
                if key not in need or need[key][1] < v:
                    need[key] = (s, v)
        e = self.engs[ename]
        seen = self.seen[ename]
        for key, (s, v) in need.items():
            if seen.get(key, 0) >= v:
                continue
            e.wait_ge(s, v)
            seen[key] = v

    def _deps(self, reads, writes):
        deps = []
        for t in reads:
            deps.append(t.w)
        for t in writes:
            deps.append(t.w)
            deps.append(t.r)
        return deps

    def op(self, ename, fn, reads=(), writes=(), pwrites=()):
        deps = self._deps(reads, writes)
        for t in pwrites:
            deps.append(t.r)
        self._wait(ename, deps)
        ins = fn(self.engs[ename])
        self.cnt[ename] += 1
        s = self.sem[ename]
        ins.then_inc(s, 1)
        v = self.cnt[ename]
        self._note(s, v)
        key = id(s)
        for t in writes:
            t.w = {key: (s, v)}
            t.r = {}
        for t in pwrites:
            t.w[key] = (s, v)
        for t in reads:
            t.r[key] = (s, v)

    def dma(self, qname, out, in_, slot, reads=(), writes=(), multi=()):
        deps = self._deps(reads, writes)
        for t in multi:
            deps.append(t.r)
        self._wait(qname, deps)
        slot = slot.k if hasattr(slot, "k") else slot
        if slot.dsem is None:
            if self.free_dsems:
                slot.dsem, slot.dcnt = self.free_dsems.pop()
            else:
                slot.dsem = self.alloc_sem("d")
                slot.dcnt = 0
            self.dtoks.append(slot)
        slot.dcnt += 16
        s, v = slot.dsem, slot.dcnt
        self.engs[qname].dma_start(out=out, in_=in_).then_inc(s, 16)
        self._note(s, v)
        key = id(s)
        for t in writes:
            t.w = {key: (s, v)}
            t.r = {}
        for t in multi:
            t.w[key] = (s, v)
            t.r = {}
        for t in reads:
            t.r[key] = (s, v)

    def barrier(self):
        allv = {k: (s, v) for k, (s, v) in self.all_sems.items() if v > 0}
        for ename in self.engs:
            self._wait(ename, [allv])
        for t in self.dtoks:
            self.free_dsems.append((t.dsem, t.dcnt))
            t.dsem = None
            t.dcnt = 0
        self.dtoks = []


class WStream:
    def __init__(self, sch, stq, wbf, qk):
        self.sch, self.stq, self.wbf, self.qk = sch, stq, wbf, qk
        self.specs = []
        self.nd = 0
        self.ncast = 0
        self.qc = 0
        self.pend = {}
        self.ce = 0

    def add(self, w_ap, K):
        self.specs.append((w_ap, K))
        return len(self.specs) - 1

    def _dma(self, i):
        w_ap, K = self.specs[i]
        kc = K // 128
        src = w_ap.rearrange("(kc p) c -> p kc c", p=128)
        qs = []
        for q0 in range(0, kc, self.qk):
            n = min(self.qk, kc - q0)
            s = self.stq[self.qc % len(self.stq)]
            self.qc += 1
            self.sch.dma("sp", s.t[:, 0:n, :], src[:, q0:q0 + n, :], s, writes=[s.k])
            qs.append((s, q0, n))
        self.pend[i] = qs

    def _cast(self, i):
        b = self.wbf[i % len(self.wbf)]
        for (s, q0, n) in self.pend.pop(i):
            eng = "dve" if self.ce % 2 == 0 else "act"
            self.ce += 1
            if eng == "dve":
                f = lambda e, s=s, q0=q0, n=n, b=b: e.tensor_copy(out=b.t[:, q0:q0 + n, :], in_=s.t[:, 0:n, :])
            else:
                f = lambda e, s=s, q0=q0, n=n, b=b: e.activation(out=b.t[:, q0:q0 + n, :], in_=s.t[:, 0:n, :],
                                                                func=AF.Copy)
            self.sch.op(eng, f, reads=[s.k], pwrites=[b.k])

    def get(self, i):
        n = len(self.specs)
        while True:
            if self.nd < min(i + 3, n) and (self.nd < 2 or self.ncast >= self.nd - 1):
                self._dma(self.nd)
                self.nd += 1
            elif self.ncast < min(i + 2, n) and self.ncast < self.nd:
                self._cast(self.ncast)
                self.ncast += 1
            else:
                break
        assert self.ncast > i
        return self.wbf[i % len(self.wbf)]


def _alibi_slopes(n):
    return np.exp2(-8.0 * np.arange(1, n + 1, dtype=np.float64) / n)


def const_layout(cfg):
    off = {}
    cur = 0

    def add(name, n):
        nonlocal cur
        off[name] = (cur, n)
        cur += n
    add("ident", 128)
    add("ones", 128)
    add("cm", 128)
    add("cmA0", 256)
    add("cmA1", 256)
    add("cmB", 256)
    add("tri", 128)
    add("En", 8 * 128)
    add("biasA", cfg.AH * 4 * 16)
    add("biasB", cfg.BH * 8 * 16)
    add("pastb", 4 * 8)
    add("past01", 4 * 8)
    add("pastb64", 64)
    add("past0164", 64)
    return off, cur


def build_consts(cfg, half):
    off, n = const_layout(cfg)
    c = np.zeros((128, n), np.float32)
    p = np.arange(128)

    def put(name, arr):
        o, m = off[name]
        c[:, o:o + m] = np.asarray(arr, np.float32).reshape(128, m)
    put("ident", np.eye(128))
    put("ones", np.ones((128, 128)))
    cm = np.where(p[:, None] <= p[None, :], 0.0, NEGB)
    put("cm", cm)
    put("cmA0", np.concatenate([cm, np.zeros((128, 128))], 1))
    put("cmA1", np.concatenate([np.full((128, 128), NEGB), cm], 1))
    put("cmB", np.concatenate([cm, cm], 1))
    put("tri", (p[:, None] <= p[None, :]).astype(np.float32))
    en = np.zeros((128, 8, 128), np.float32)
    for nb in range(8):
        en[nb, nb, :] = 1.0
    put("En", en)
    slopes = _alibi_slopes(cfg.NAL)
    sA = slopes[cfg.BH:]
    sB = slopes[:cfg.BH]
    kvalid = np.zeros(16)
    if half == 0:
        kvalid[:8] = NEGB
    bA = np.zeros((128, cfg.AH, 4, 16))
    for h in range(cfg.AH):
        for qb in range(4):
            ref = 1024 + qb * 256 + 128
            for kt in range(16):
                bA[:, h, qb, kt] = sA[h] * (kt * 128 + p - ref) + kvalid[kt]
    put("biasA", bA)
    bB = np.zeros((128, cfg.BH, 8, 16))
    for h in range(cfg.BH):
        for qt in range(8):
            ref = 1024 + qt * 128 + 64
            for kt in range(16):
                bB[:, h, qt, kt] = sB[h] * (kt * 128 + p - ref) + kvalid[kt]
    put("biasB", bB)
    pb = np.zeros((128, 4, 8))
    p01 = np.zeros((128, 4, 8))
    for qb in range(4):
        nq = 4 + qb
        for nb in range(8):
            ok = (nb < nq) and (half == 1 or nb >= 4)
            pb[:, qb, nb] = 0.0 if ok else -1e30
            p01[:, qb, nb] = 1.0 if ok else 0.0
    put("pastb", pb)
    put("past01", p01)
    put("pastb64", np.repeat(pb, 2, axis=1))
    put("past0164", np.repeat(p01, 2, axis=1))
    return c


class Tile:
    def __init__(self, t, name):
        self.t = t
        self.k = Tok(name)


def build_program(cfg, layer_ids):
    D, T, BW = cfg.D, cfg.T, cfg.BW
    nc = bass.Bass("TRN2", target_bir_lowering=False)
    coff, cn = const_layout(cfg)

    def din(name, shape, dt=F32):
        return nc.dram_tensor(name, list(shape), dt, kind="ExternalInput").ap()

    def dscr(name, shape, dt=BF16):
        return nc.dram_tensor(name, list(shape), dt, kind="Internal").ap()

    A = {}
    A["mem"] = din("mem", [256, D])
    A["cst_d"] = din("cst", [128, cn])
    A["fg_d"] = din("fgvec", [128, D])
    out_d = nc.dram_tensor("out", [T, D], F32, kind="ExternalOutput").ap()
    A["pT"] = dscr("pT", [8 * BW, T])
    A["kTa"] = dscr("kTa", [BW, 2048])
    A["kTb"] = dscr("kTb", [BW, 2048])
    A["va_s"] = dscr("va_s", [2048, BW])
    A["vb_s"] = dscr("vb_s", [2048, BW])
    A["vc_s"] = dscr("vc_s", [T, BW])
    A["mkT"] = dscr("mkT", [BW, 256])
    A["mv_s"] = dscr("mv_s", [256, BW])
    A["mgT"] = dscr("mgT", [D, T])
    A["xscr"] = dscr("xscr", [T, D], F32)
    xo = din("xo", [T, D])
    sch = Sched(nc)
    for idx, l in enumerate(layer_ids):
        last = idx == len(layer_ids) - 1
        final = l == cfg.depth - 1
        sfx = f"_{l}"
        A["w_in"] = din("w_in" + sfx, [D, cfg.WIN])
        A["w_mkv"] = din("w_mkv" + sfx, [D, 2 * BW])
        A["w_br"] = din("w_br" + sfx, [4 * BW, D])
        A["w_out"] = din("w_out" + sfx, [D, D])
        A["gv_d"] = din("gvec" + sfx, [128, D])
        A["mgv_d"] = din("mgvec" + sfx, [128, D])
        A["lam_d"] = din("lamv" + sfx, [128, 4 * 128])
        A["subg_d"] = din("subg" + sfx, [128, 2])
        A["sgug_d"] = din("sgug" + sfx, [128, BW])
        A["sgub_d"] = din("sgub" + sfx, [128, BW])
        A["sguw_d"] = din("sguwT" + sfx, [128, cfg.SG * 128])
        A["sgubs_d"] = din("sgubs" + sfx, [128, cfg.SG * 128])
        A["kv_own"] = [nc.dram_tensor(f"kvown{i}{sfx}", sh, BF16) for i, sh in
                       enumerate([[BW, T], [BW, T], [T, BW], [T, BW]])]
        A["kv_all"] = [nc.dram_tensor(f"kvall{i}{sfx}", [2 * sh[0], sh[1]], BF16) for i, sh in
                       enumerate([[BW, T], [BW, T], [T, BW], [T, BW]])]
        A["xo"] = xo
        if last:
            A["dest"] = out_d
        else:
            x1_t = nc.dram_tensor(f"x1buf{sfx}", [T, D], F32)
            A["dest"] = x1_t.ap()
            xo = x1_t.ap()
        emit_layer(nc, sch, cfg, l, final, A, f"L{l}_", None)
        sch.barrier()
    return nc


def emit_layer(nc, sch, cfg, layer_idx, final, A, pfx, tk_x):
    D, T, KC, BW, BC = cfg.D, cfg.T, cfg.KC, cfg.BW, cfg.BC
    coff, cn = const_layout(cfg)
    lam_init = 0.8 - 0.6 * math.exp(-0.3 * layer_idx)
    xo, mem = A["xo"], A["mem"]
    kvo = [t_.ap() for t_ in A["kv_own"]]
    kva = [t_.ap() for t_ in A["kv_all"]]
    w_in, w_mkv, w_br, w_out = A["w_in"], A["w_mkv"], A["w_br"], A["w_out"]
    gv_d, mgv_d, fg_d, lam_d, subg_d = A["gv_d"], A["mgv_d"], A["fg_d"], A["lam_d"], A["subg_d"]
    sgug_d, sgub_d, sguw_d, sgubs_d, cst_d = A["sgug_d"], A["sgub_d"], A["sguw_d"], A["sgubs_d"], A["cst_d"]
    out_d = A["dest"]
    pT, kTa, kTb, va_s, vb_s, vc_s, mkT, mv_s, mgT = (A[k] for k in ("pT", "kTa", "kTb", "va_s", "vb_s", "vc_s",
                                                                      "mkT", "mv_s", "mgT"))
    PQA, PZA, PQB, PZB, PUC, PZC, PQM, PZM = [i * BW for i in range(8)]
    xout = A["xscr"] if final else out_d
    XRD = [tk_x] if tk_x is not None else []
    tk_scr = Tok("scr")
    tk_kv = Tok("kv")
    tk_kvall = Tok("kvall")
    tk_mg = Tok("mg")
    tk_xout = Tok("xout")

    from contextlib import ExitStack

    def alloc(es, name, shape, dt):
        return Tile(es.enter_context(nc.sbuf_tensor(pfx + "sb_" + name, list(shape), dt)), name)

    def palloc(es, name, shape, dt):
        return Tile(es.enter_context(nc.psum_tensor(pfx + "ps_" + name, list(shape), dt)), name)

    with ExitStack() as es0:
        hT = alloc(es0, "hT", [128, KC, T], BF16)

        with ExitStack() as es:
            xs = [alloc(es, f"xs{i}", [128, D], F32) for i in range(2)]
            xn = [alloc(es, f"xn{i}", [128, D], BF16) for i in range(2)]
            gvt = alloc(es, "gvt", [128, D], F32)
            hmT = alloc(es, "hmT", [128, KC, 256], BF16)
            cid = alloc(es, "cid", [128, 128], F32)
            sch.dma("sp", cid.t[:], cst_d[:, coff["ident"][0]:coff["ident"][0] + 128], cid, writes=[cid.k])
            st = [alloc(es, f"st{i}", [128, 4]) if False else alloc(es, f"st{i}", [128, 4], F32) for i in range(2)]
            QK = min(8, KC)
            stq = [alloc(es, f"stq{i}", [128, QK, 128], F32) for i in range(8)]
            wbf = [alloc(es, f"wbf{i}", [128, KC, 128], BF16) for i in range(3)]
            wsm = WStream(sch, stq, wbf, QK)
            otl = [alloc(es, f"otl{i}", [128, T], BF16) for i in range(3)]
            identb = alloc(es, "identb", [128, 128], BF16)
            ptr = [palloc(es, f"ptr{i}", [128, 1024], BF16) for i in range(2)]
            pacc = [palloc(es, f"pacc{i}", [128, 512], F32) for i in range(4)]
            sch.op("dve", lambda e: e.tensor_copy(out=identb.t[:], in_=cid.t[:]), reads=[cid.k], writes=[identb.k])
            cnt = {"x": 0, "w": 0, "wb": 0, "ot": 0, "pa": 0, "pt": 0}

            def norm_transpose(x_ap, ntok, gsrc, hdst):
                sch.dma("sp", gvt.t[:], gsrc[:, :], gvt, writes=[gvt.k])
                def stage_a(i):
                        a = xs[cnt["x"] % 2]
                        b = xn[cnt["x"] % 2]
                        s4 = st[cnt["x"] % 2]
                        cnt["x"] += 1
                        sch.dma("sp", a.t[:], x_ap(i) if callable(x_ap) else x_ap[i * 128:(i + 1) * 128, :], a, reads=XRD, writes=[a.k])
                        sch.op("act", lambda e: e.activation(out=b.t[:], in_=a.t[:], func=AF.Square,
                                                             accum_out=s4.t[:, 0:1]),
                               reads=[a.k], writes=[b.k, s4.k])
                        sch.op("dve", lambda e: e.tensor_scalar(out=s4.t[:, 1:2], in0=s4.t[:, 0:1], scalar1=1.0 / D,
                                                                scalar2=RMS_EPS, op0=ALU.mult, op1=ALU.add),
                               reads=[s4.k], writes=[s4.k])
                        sch.op("act", lambda e: e.activation(out=s4.t[:, 2:3], in_=s4.t[:, 1:2], func=AF.Sqrt),
                               reads=[s4.k], writes=[s4.k])
                        sch.op("dve", lambda e: e.reciprocal(out=s4.t[:, 3:4], in_=s4.t[:, 2:3]),
                               reads=[s4.k], writes=[s4.k])
                        sch.op("dve", lambda e: e.scalar_tensor_tensor(out=b.t[:], in0=a.t[:], scalar=s4.t[:, 3:4],
                                                                       in1=gvt.t[:], op0=ALU.mult, op1=ALU.mult),
                               reads=[a.k, s4.k, gvt.k], writes=[b.k])

                        return b

                def stage_b(i, b):
                        for g8 in range(KC // 8):
                            ps = ptr[cnt["pt"] % 2]
                            cnt["pt"] += 1

                            def tr(e, ps=ps, g8=g8, b=b):
                                ins = None
                                for j in range(8):
                                    kc = g8 * 8 + j
                                    ins = e.transpose(out=ps.t[:, j * 128:(j + 1) * 128],
                                                      in_=b.t[:, kc * 128:(kc + 1) * 128], identity=identb.t[:])
                                return ins
                            sch.op("pe", tr, reads=[b.k, identb.k], writes=[ps.k])
                            eng = "dve" if g8 % 2 == 0 else "act"
                            if eng == "dve":
                                f = lambda e, ps=ps, g8=g8, i=i: e.tensor_copy(
                                    out=hdst.t[:, g8 * 8:(g8 + 1) * 8, i * 128:(i + 1) * 128],
                                    in_=ps.t[:].rearrange("p (j t) -> p j t", t=128))
                            else:
                                f = lambda e, ps=ps, g8=g8, i=i: e.activation(
                                    out=hdst.t[:, g8 * 8:(g8 + 1) * 8, i * 128:(i + 1) * 128],
                                    in_=ps.t[:].rearrange("p (j t) -> p j t", t=128), func=AF.Copy)
                            sch.op(eng, f, reads=[ps.k], writes=[hdst.k])

                nt_ = ntok // 128
                pend_b = stage_a(0)
                for i in range(nt_):
                    cur = pend_b
                    if i + 1 < nt_:
                        pend_b = stage_a(i + 1)
                    stage_b(i, cur)

            def sweep(src_tile, nkc, ntok, wbase, nchunks, mode, dest, gtok):
                for j in range(nchunks):
                    wb = wsm.get(wbase + j)
                    ot = otl[cnt["ot"] % 3]
                    cnt["ot"] += 1
                    if mode == "fm":
                        nh = max(1, ntok // 512)
                        n = ntok // nh
                        for hh in range(nh):
                            ps = pacc[cnt["pa"] % 4]
                            cnt["pa"] += 1

                            def mm(e, ps=ps, hh=hh, n=n, wb=wb):
                                ins = None
                                for k in range(nkc):
                                    ins = e.matmul(ps.t[:, 0:n], lhsT=wb.t[:, k, :],
                                                   rhs=src_tile.t[:, k, hh * n:(hh + 1) * n],
                                                   start=(k == 0), stop=(k == nkc - 1))
                                return ins
                            sch.op("pe", mm, reads=[wb.k, src_tile.k], writes=[ps.k])
                            eng = "dve" if hh % 2 == 0 else "act"
                            if eng == "dve":
                                f = lambda e, ps=ps, hh=hh, n=n, ot=ot: e.tensor_copy(
                                    out=ot.t[:, hh * n:(hh + 1) * n], in_=ps.t[:, 0:n])
                            else:
                                f = lambda e, ps=ps, hh=hh, n=n, ot=ot: e.activation(
                                    out=ot.t[:, hh * n:(hh + 1) * n], in_=ps.t[:, 0:n], func=AF.Copy)
                            sch.op(eng, f, reads=[ps.k], writes=[ot.k])
                        sch.dma("sp", dest(j), ot.t[:, 0:ntok], ot, reads=[ot.k], multi=[gtok])
                    else:
                        nt = ntok // 128
                        for g4 in range((nt + 3) // 4):
                            ps = pacc[cnt["pa"] % 4]
                            cnt["pa"] += 1
                            m = min(4, nt - g4 * 4)

                            def mm(e, ps=ps, g4=g4, m=m, wb=wb):
                                ins = None
                                for ii in range(m):
                                    i = g4 * 4 + ii
                                    for k in range(nkc):
                                        ins = e.matmul(ps.t[:, ii * 128:(ii + 1) * 128],
                                                       lhsT=src_tile.t[:, k, i * 128:(i + 1) * 128],
                                                       rhs=wb.t[:, k, :], start=(k == 0), stop=(k == nkc - 1))
                                return ins
                            sch.op("pe", mm, reads=[wb.k, src_tile.k], writes=[ps.k])
                            eng = "dve" if g4 % 2 == 0 else "act"
                            if eng == "dve":
                                f = lambda e, ps=ps, g4=g4, m=m, ot=ot: e.tensor_copy(
                                    out=ot.t[:, g4 * 512:g4 * 512 + m * 128], in_=ps.t[:, 0:m * 128])
                            else:
                                f = lambda e, ps=ps, g4=g4, m=m, ot=ot: e.activation(
                                    out=ot.t[:, g4 * 512:g4 * 512 + m * 128], in_=ps.t[:, 0:m * 128], func=AF.Copy)
                            sch.op(eng, f, reads=[ps.k], writes=[ot.k])
                        with nc.allow_non_contiguous_dma(reason="token-major scratch rows"):
                            sch.dma("sp", dest(j).rearrange("(i p) c -> p i c", p=128),
                                    ot.t[:, 0:ntok].rearrange("p (i c) -> p i c", c=128), ot,
                                    reads=[ot.k], multi=[gtok])

            def wcol(w_ap, c0):
                return lambda j: w_ap[:, c0 + j * 128:c0 + (j + 1) * 128]

            fm_blocks = [(0, PQA), (3, PZA), (4, PQB), (7, PZB), (8, PUC), (10, PZC), (11, PQM), (12, PZM)]
            plan = [("norm", mem, 256, mgv_d, hmT),
                    ("norm", xo, T, gv_d, hT),
                    ("sw", 256, w_mkv, 0, "fm", lambda j: mkT[j * 128:(j + 1) * 128, :], tk_scr, hmT),
                    ("sw", 256, w_mkv, BW, "tm", lambda j: mv_s[:, j * 128:(j + 1) * 128], tk_scr, hmT),
                    ("sw", T, w_in, 1 * BW, "fm", lambda j: kvo[0][j * 128:(j + 1) * 128, :], tk_kv, hT),
                    ("sw", T, w_in, 5 * BW, "fm", lambda j: kvo[1][j * 128:(j + 1) * 128, :], tk_kv, hT),
                    ("sw", T, w_in, 2 * BW, "tm", lambda j: kvo[2][:, j * 128:(j + 1) * 128], tk_kv, hT),
                    ("sw", T, w_in, 6 * BW, "tm", lambda j: kvo[3][:, j * 128:(j + 1) * 128], tk_kv, hT),
                    ("xchg",)]
            for blk, prow in fm_blocks:
                plan.append(("sw", T, w_in, blk * BW, "fm",
                             lambda j, prow=prow: pT[prow + j * 128:prow + (j + 1) * 128, :], tk_scr, hT))
            plan.append(("sw", T, w_in, 9 * BW, "tm", lambda j: vc_s[:, j * 128:(j + 1) * 128], tk_scr, hT))
            bases = []
            for it in plan:
                if it[0] == "sw":
                    bases.append(len(wsm.specs))
                    for j in range(BC):
                        wsm.add(it[2][:, it[3] + j * 128:it[3] + (j + 1) * 128], D)
                else:
                    bases.append(None)
            for it, wbase in zip(plan, bases):
                if it[0] == "norm":
                    norm_transpose(it[1], it[2], it[3], it[4])
                elif it[0] == "xchg":
                    sch._wait("pool", [tk_kv.w])
                    csem = sch.alloc_sem("cc")
                    groups = [[2 * i, 2 * i + 1] for i in range(cfg.ncores // 2)]
                    for i in range(4):
                        cc = nc.gpsimd.collective_compute("AllGather", ALU.bypass, replica_groups=groups,
                                                          ins=[A["kv_own"][i].ap().opt()],
                                                          outs=[A["kv_all"][i].ap().opt()])
                        cc.then_inc(csem)
                    sch._note(csem, 4)
                    tk_kvall.w = {id(csem): (csem, 4)}
                else:
                    sweep(it[7], KC, it[1], wbase, BC, it[4], it[5], it[6])
            sch.barrier()

        with ExitStack() as es1:
            yT = alloc(es1, "yT", [128, 4 * BC, T], BF16)
            with ExitStack() as es:
                build_mixers(nc, sch, cfg, es, alloc, palloc, cst_d, coff, cn, yT, tk_scr, lam_init,
                             dict(pT=pT, kvo=kvo, kva=kva, tk_kv=tk_kv, tk_kvall=tk_kvall, vc=vc_s, mkT=mkT, mv=mv_s,
                                  PQA=PQA, PZA=PZA, PQB=PQB, PZB=PZB, PUC=PUC, PZC=PZC, PQM=PQM, PZM=PZM,
                                  lam=lam_d, subg=subg_d, sgug=sgug_d, sgub=sgub_d, sguw=sguw_d, sgubs=sgubs_d))
                sch.barrier()
            with ExitStack() as es:
                QK = min(8, KC)
                stq = [alloc(es, f"gstq{i}", [128, QK, 128], F32) for i in range(8)]
                wbf = [alloc(es, f"gwbf{i}", [128, KC, 128], BF16) for i in range(3)]
                wsm = WStream(sch, stq, wbf, QK)
                sg = [alloc(es, f"sg{i}", [128, T], F32) for i in range(2)]
                acc = [alloc(es, f"acc{i}", [128, T], F32) for i in range(1)]
                tmp = [alloc(es, f"tmp{i}", [128, T], F32) for i in range(1)]
                mo = [alloc(es, f"mo{i}", [128, T], BF16) for i in range(2)]
                pg = [palloc(es, f"pg{i}", [128, 512], F32) for i in range(4)]
                pb = [palloc(es, f"pb{i}", [128, 512], F32) for i in range(4)]
                cnt = {"w": 0, "wb": 0, "pg": 0, "pb": 0, "sg": 0, "tmp": 0}

                specs = []
                for c in range(KC):
                    for b in range(4):
                        wsm.add(w_in[:, 13 * BW + b * D + c * 128:13 * BW + b * D + (c + 1) * 128], D)
                        wsm.add(w_br[b * BW:(b + 1) * BW, c * 128:(c + 1) * 128], BW)
                getw = wsm.get
                for c in range(KC):
                    ac = acc[0]
                    for b in range(4):
                        wg = getw((c * 4 + b) * 2)
                        sgt = sg[cnt["sg"] % 2]
                        cnt["sg"] += 1
                        for hh in range(2):
                            ps = pg[cnt["pg"] % 4]
                            cnt["pg"] += 1

                            def mm(e, ps=ps, hh=hh, wg=wg):
                                ins = None
                                for k in range(KC):
                                    ins = e.matmul(ps.t[:, :], lhsT=wg.t[:, k, :], rhs=hT.t[:, k, hh * 512:(hh + 1) * 512],
                                                   start=(k == 0), stop=(k == KC - 1))
                                return ins
                            sch.op("pe", mm, reads=[wg.k, hT.k], writes=[ps.k])
                            sch.op("act", lambda e, ps=ps, hh=hh, sgt=sgt: e.activation(
                                out=sgt.t[:, hh * 512:(hh + 1) * 512], in_=ps.t[:, :], func=AF.Sigmoid),
                                reads=[ps.k], writes=[sgt.k])
                        wb_ = getw((c * 4 + b) * 2 + 1)
                        for hh in range(2):
                            ps = pb[cnt["pb"] % 4]
                            cnt["pb"] += 1

                            def mm2(e, ps=ps, hh=hh, wb_=wb_, b=b):
                                ins = None
                                for k in range(BC):
                                    ins = e.matmul(ps.t[:, :], lhsT=wb_.t[:, k, :],
                                                   rhs=yT.t[:, b * BC + k, hh * 512:(hh + 1) * 512],
                                                   start=(k == 0), stop=(k == BC - 1))
                                return ins
                            sch.op("pe", mm2, reads=[wb_.k, yT.k], writes=[ps.k])
                            sl = slice(hh * 512, (hh + 1) * 512)
                            if b == 0:
                                sch.op("dve", lambda e, ps=ps, sl=sl, sgt=sgt, ac=ac: e.tensor_tensor(
                                    out=ac.t[:, sl], in0=ps.t[:, :], in1=sgt.t[:, sl], op=ALU.mult),
                                    reads=[ps.k, sgt.k], writes=[ac.k])
                            else:
                                tm_ = tmp[0]
                                cnt["tmp"] += 1
                                sch.op("dve", lambda e, ps=ps, sl=sl, sgt=sgt, tm_=tm_: e.tensor_tensor(
                                    out=tm_.t[:, sl], in0=ps.t[:, :], in1=sgt.t[:, sl], op=ALU.mult),
                                    reads=[ps.k, sgt.k], writes=[tm_.k])
                                if b < 3:
                                    sch.op("pool", lambda e, sl=sl, tm_=tm_, ac=ac: e.tensor_tensor(
                                        out=ac.t[:, sl], in0=ac.t[:, sl], in1=tm_.t[:, sl], op=ALU.add),
                                        reads=[tm_.k, ac.k], writes=[ac.k])
                                else:
                                    m_ = mo[c % 2]
                                    sch.op("pool", lambda e, sl=sl, tm_=tm_, ac=ac, m_=m_: e.tensor_tensor(
                                        out=m_.t[:, sl], in0=ac.t[:, sl], in1=tm_.t[:, sl], op=ALU.add),
                                        reads=[tm_.k, ac.k], writes=[m_.k])
                    m_ = mo[c % 2]
                    sch.dma("sp", mgT[c * 128:(c + 1) * 128, :], m_.t[:, :], m_, reads=[m_.k], multi=[tk_mg])
                sch.barrier()
            with ExitStack() as es:
                QK = min(8, KC)
                stq = [alloc(es, f"ostq{i}", [128, QK, 128], F32) for i in range(8)]
                wbf = [alloc(es, f"owbf{i}", [128, KC, 128], BF16) for i in range(3)]
                wsm = WStream(sch, stq, wbf, QK)
                for j in range(KC):
                    wsm.add(w_out[:, j * 128:(j + 1) * 128], D)
                xt = [alloc(es, f"xt{i}", [128, 8, 128], F32) for i in range(2)]
                ob = [alloc(es, f"ob{i}", [128, 8, 128], F32) for i in range(2)]
                po = [palloc(es, f"po{i}", [128, 512], F32) for i in range(4)]
                for q in range(4):
                    kq = KC // 4
                    sch.dma("sp", hT.t[:, q * kq:(q + 1) * kq, :],
                            mgT[q * kq * 128:(q + 1) * kq * 128, :].rearrange("(kc p) t -> p kc t", p=128),
                            hT, reads=[tk_mg], multi=[hT.k])
                cp = 0

                for j in range(KC):
                    b = wsm.get(j)
                    x_ = xt[j % 2]
                    o_ = ob[j % 2]
                    with nc.allow_non_contiguous_dma(reason="column block of x"):
                        sch.dma("sp", x_.t[:], xo[:, j * 128:(j + 1) * 128].rearrange("(i p) c -> p i c", p=128),
                                x_, reads=XRD, writes=[x_.k])
                    for g4 in range(2):
                        ps = po[cp % 4]
                        cp += 1

                        def mm(e, ps=ps, g4=g4, b=b):
                            ins = None
                            for ii in range(4):
                                i = g4 * 4 + ii
                                for k in range(KC):
                                    ins = e.matmul(ps.t[:, ii * 128:(ii + 1) * 128],
                                                   lhsT=hT.t[:, k, i * 128:(i + 1) * 128], rhs=b.t[:, k, :],
                                                   start=(k == 0), stop=(k == KC - 1))
                            return ins
                        sch.op("pe", mm, reads=[b.k, hT.k], writes=[ps.k])
                        sch.op("dve", lambda e, ps=ps, g4=g4, x_=x_, o_=o_: e.tensor_tensor(
                            out=o_.t[:, g4 * 4:(g4 + 1) * 4, :], in0=ps.t[:].rearrange("p (i c) -> p i c", c=128),
                            in1=x_.t[:, g4 * 4:(g4 + 1) * 4, :], op=ALU.add),
                            reads=[ps.k, x_.k], writes=[o_.k])
                    with nc.allow_non_contiguous_dma(reason="column block of out"):
                        sch.dma("sp", xout[:, j * 128:(j + 1) * 128].rearrange("(i p) c -> p i c", p=128), o_.t[:],
                                o_, reads=[o_.k], multi=[tk_xout])
                sch.barrier()
            if final:
                with ExitStack() as es:
                    fx = [alloc(es, f"fx{i}", [128, D], F32) for i in range(2)]
                    fgt = alloc(es, "fgt", [128, D], F32)
                    fj = alloc(es, "fj", [128, D], BF16)
                    fs = [alloc(es, f"fs{i}", [128, 4], F32) for i in range(2)]
                    sch.dma("sp", fgt.t[:], fg_d[:, :], fgt, writes=[fgt.k])
                    for i in range(T // 128):
                        a = fx[i % 2]
                        o_ = a
                        s4 = fs[i % 2]
                        sch.dma("sp", a.t[:], xout[i * 128:(i + 1) * 128, :], a, reads=[tk_xout], writes=[a.k])
                        sch.op("act", lambda e, a=a, s4=s4: e.activation(out=fj.t[:], in_=a.t[:], func=AF.Square,
                                                                         accum_out=s4.t[:, 0:1]),
                               reads=[a.k], writes=[fj.k, s4.k])
                        sch.op("dve", lambda e, s4=s4: e.tensor_scalar(out=s4.t[:, 1:2], in0=s4.t[:, 0:1],
                                                                       scalar1=1.0 / D, scalar2=RMS_EPS,
                                                                       op0=ALU.mult, op1=ALU.add),
                               reads=[s4.k], writes=[s4.k])
                        sch.op("act", lambda e, s4=s4: e.activation(out=s4.t[:, 2:3], in_=s4.t[:, 1:2], func=AF.Sqrt),
                               reads=[s4.k], writes=[s4.k])
                        sch.op("dve", lambda e, s4=s4: e.reciprocal(out=s4.t[:, 3:4], in_=s4.t[:, 2:3]),
                               reads=[s4.k], writes=[s4.k])
                        sch.op("dve", lambda e, a=a, s4=s4, o_=o_: e.scalar_tensor_tensor(
                            out=o_.t[:], in0=a.t[:], scalar=s4.t[:, 3:4], in1=fgt.t[:], op0=ALU.mult, op1=ALU.mult),
                            reads=[a.k, s4.k, fgt.k], writes=[o_.k])
                        sch.dma("sp", out_d[i * 128:(i + 1) * 128, :], o_.t[:], o_, reads=[o_.k])
                    sch.barrier()


def build_mixers(nc, sch, cfg, es, alloc, palloc, cst_d, coff, cn, yT, tk_scr, lam_init, d):
    from contextlib import ExitStack
    cst = alloc(es, "cst", [128, cn], F32)
    sch.dma("sp", cst.t[:], cst_d[:, :], cst, writes=[cst.k])

    def cv(name, a=0, b=None):
        o, m = coff[name]
        b = m if b is None else b
        return cst.t[:, o + a:o + b]
    T, BW, BC = cfg.T, cfg.BW, cfg.BC
    pT = d["pT"]
    RD = [tk_scr]
    RKV = [d["tk_kv"]]
    RKA = [d["tk_kvall"]]

    def ncd():
        return nc.allow_non_contiguous_dma(reason="scratch layout")

    identb = alloc(es, "m_identb", [128, 128], BF16)
    onesb = alloc(es, "m_onesb", [128, 128], BF16)
    cmA0 = alloc(es, "m_cmA0", [128, 256], BF16)
    cmA1 = alloc(es, "m_cmA1", [128, 256], BF16)
    cmB = alloc(es, "m_cmB", [128, 256], BF16)
    Enb = alloc(es, "m_En", [128, 8 * 128], BF16)
    for tl, nm in ((identb, "ident"), (onesb, "ones"), (cmA0, "cmA0"), (cmA1, "cmA1"), (cmB, "cmB"), (Enb, "En")):
        sch.op("dve", lambda e, tl=tl, nm=nm: e.tensor_copy(out=tl.t[:], in_=cv(nm)), reads=[cst.k], writes=[tl.k])
    lamt = alloc(es, "m_lamt", [128, 4 * 128], F32)
    lsc = alloc(es, "m_lsc", [128, 8], F32)
    ljunk = alloc(es, "m_ljunk", [128, 128], F32)
    subg = alloc(es, "m_subg", [128, 2], F32)
    sch.dma("sp", lamt.t[:], d["lam"][:, :], lamt, writes=[lamt.k])
    sch.dma("sp", subg.t[:], d["subg"][:, :], subg, writes=[subg.k])
    for i in range(2):
        sch.op("dve", lambda e, i=i: e.tensor_tensor(out=ljunk.t[:], in0=lamt.t[:, (2 * i) * 128:(2 * i + 1) * 128],
                                                     in1=lamt.t[:, (2 * i + 1) * 128:(2 * i + 2) * 128], op=ALU.mult),
               reads=[lamt.k], writes=[ljunk.k])
        sch.op("dve", lambda e, i=i: e.reduce_sum(out=lsc.t[:, i:i + 1], in_=ljunk.t[:], axis=AX.X),
               reads=[ljunk.k], writes=[lsc.k])
    sch.op("act", lambda e: e.activation(out=lsc.t[:, 2:4], in_=lsc.t[:, 0:2], func=AF.Exp), reads=[lsc.k], writes=[lsc.k])
    sch.op("dve", lambda e: e.scalar_tensor_tensor(out=lsc.t[:, 4:5], in0=lsc.t[:, 3:4], scalar=-lam_init,
                                                   in1=lsc.t[:, 2:3], op0=ALU.add, op1=ALU.subtract),
           reads=[lsc.k], writes=[lsc.k])
    sch.op("dve", lambda e: e.tensor_scalar(out=subg.t[:], in0=subg.t[:], scalar1=(1.0 - lam_init), scalar2=None,
                                            op0=ALU.mult), reads=[subg.k], writes=[subg.k])
    neglam = lsc.t[:, 4:5]

    pS = [palloc(es, f"pS{i}", [128, 512], F32) for i in range(2)]
    pO = [palloc(es, f"pO{i}", [128, 512], F32) for i in range(2)]
    pR = [palloc(es, f"pR{i}", [128, 512], F32) for i in range(2)]
    pG = palloc(es, "pG", [128, 512], F32)
    pTr = palloc(es, "pTr", [128, 1024], BF16)

    pt = [alloc(es, f"m_pt{i}", [128, 512], BF16) for i in range(3)]
    rinv = [alloc(es, f"m_rinv{i}", [128, 512], F32) for i in range(2)]
    t1 = [alloc(es, f"m_t1{i}", [128, 512], F32) for i in range(2)]
    c_ = {"pt": 0, "s": 0, "o": 0, "ld": 0}

    esb = ExitStack()
    scaleA = 128.0 ** -0.5
    qa = [alloc(esb, f"a_q{i}", [128, T], BF16) for i in range(2)]
    ka = [alloc(esb, f"a_k{i}", [128, 2048], BF16) for i in range(2)]
    vA = [alloc(esb, f"a_v{i}", [128, 16, 128], BF16) for i in range(2)]
    za = [alloc(esb, f"a_z{i}", [128, T], BF16) for i in range(2)]
    sz = [alloc(esb, f"a_sz{i}", [128, T], BF16) for i in range(2)]
    km32 = alloc(esb, "a_km32", [128, 8], F32)
    kmhl = [alloc(esb, f"a_kmhl{i}", [128, 16], BF16) for i in range(2)]
    kmh32 = alloc(esb, "a_kmh32", [128, 8], F32)
    g64 = alloc(esb, "a_g64", [128, 64], F32)
    top64 = alloc(esb, "a_top64", [128, 64], F32)
    sel64 = alloc(esb, "a_sel64", [128, 64], F32)
    selq = alloc(esb, "a_selq", [128, 64], BF16)
    selT = [alloc(esb, f"a_selT{i}", [128, T], BF16) for i in range(2)]
    for st0 in selT:
        sch.op("dve", lambda e, st0=st0: e.memset(st0.t[:], 0.0), writes=[st0.k])

    def a_part1(h):
        q_, k_, v_, z_, s_ = qa[h % 2], ka[h % 2], vA[h % 2], za[h % 2], sz[h % 2]
        km_ = kmhl[h % 2]
        r0 = h * 128
        sch.dma("sp", q_.t[:], pT[d["PQA"] + r0:d["PQA"] + r0 + 128, :], q_, reads=RD, writes=[q_.k])
        sch.dma("sp", k_.t[:, 0:T], d["kva"][0][r0:r0 + 128, :], k_, reads=RKA, writes=[k_.k])
        sch.dma("sp", k_.t[:, T:2 * T], d["kvo"][0][r0:r0 + 128, :], k_, reads=RKV, multi=[k_.k])
        with ncd():
            sch.dma("sp", v_.t[:, 0:8, :], d["kva"][2][0:T, r0:r0 + 128].rearrange("(kt p) c -> p kt c", p=128), v_,
                    reads=RKA, writes=[v_.k])
            sch.dma("sp", v_.t[:, 8:16, :], d["kvo"][2][:, r0:r0 + 128].rearrange("(kt p) c -> p kt c", p=128), v_,
                    reads=RKV, multi=[v_.k])
        sch.dma("sp", z_.t[:], pT[d["PZA"] + r0:d["PZA"] + r0 + 128, :], z_, reads=RD, writes=[z_.k])
        sch.op("act", lambda e: e.activation(out=s_.t[:], in_=z_.t[:], func=AF.Silu), reads=[z_.k], writes=[s_.k])
        sch.op("dve", lambda e: e.tensor_reduce(out=km32.t[:], in_=k_.t[:].rearrange("p (n s) -> p n s", s=256),
                                                axis=AX.X, op=ALU.add), reads=[k_.k], writes=[km32.k])
        sch.op("dve", lambda e: e.tensor_copy(out=km_.t[:, 0:8], in_=km32.t[:]), reads=[km32.k], writes=[km_.k])
        sch.op("dve", lambda e: e.tensor_copy(out=kmh32.t[:], in_=km_.t[:, 0:8]), reads=[km_.k], writes=[kmh32.k])
        sch.op("dve", lambda e: e.tensor_tensor(out=km_.t[:, 8:16], in0=km32.t[:], in1=kmh32.t[:], op=ALU.subtract),
               reads=[km32.k, kmh32.k, km_.k], writes=[km_.k])

        def gm(e):
            ins = None
            for qt in range(8):
                e.matmul(pG.t[:, qt * 8:qt * 8 + 8], lhsT=q_.t[:, qt * 128:(qt + 1) * 128], rhs=km_.t[:, 0:8],
                         start=True, stop=False)
                ins = e.matmul(pG.t[:, qt * 8:qt * 8 + 8], lhsT=q_.t[:, qt * 128:(qt + 1) * 128], rhs=km_.t[:, 8:16],
                               start=False, stop=True)
            return ins
        sch.op("pe", gm, reads=[q_.k, km_.k], writes=[pG.k])
        sch.op("dve", lambda e: e.tensor_tensor(out=g64.t[:], in0=pG.t[:, 0:64], in1=cv("pastb64"), op=ALU.add),
               reads=[pG.k, cst.k], writes=[g64.k])

        def mx(e):
            ins = None
            for qt in range(8):
                ins = e.max(out=top64.t[:, qt * 8:qt * 8 + 8], in_=g64.t[:, qt * 8:qt * 8 + 8])
            return ins
        sch.op("dve", mx, reads=[g64.k], writes=[top64.k])

        def ge(e):
            ins = None
            for qt in range(8):
                ins = e.tensor_scalar(out=sel64.t[:, qt * 8:qt * 8 + 8], in0=g64.t[:, qt * 8:qt * 8 + 8],
                                      scalar1=top64.t[:, qt * 8 + 2:qt * 8 + 3], scalar2=None, op0=ALU.is_ge)
            return ins
        sch.op("dve", ge, reads=[g64.k, top64.k], writes=[sel64.k])
        sch.op("dve", lambda e: e.tensor_tensor(out=sel64.t[:], in0=sel64.t[:], in1=cv("past0164"), op=ALU.mult),
               reads=[sel64.k, cst.k], writes=[sel64.k])
        sch.op("dve", lambda e: e.tensor_scalar(out=selq.t[:], in0=sel64.t[:], scalar1=1.0, scalar2=-NEGB,
                                                op0=ALU.subtract, op1=ALU.mult), reads=[sel64.k], writes=[selq.k])

    def a_part2(h):
        st_ = selT[h % 2]

        def tr(e):
            ins = None
            for qt in range(8):
                ins = e.transpose(out=pTr.t[0:8, qt * 128:(qt + 1) * 128], in_=selq.t[:, qt * 8:qt * 8 + 8],
                                  identity=identb.t[:])
            return ins
        sch.op("pe", tr, reads=[selq.k, identb.k], writes=[pTr.k])
        sch.op("dve", lambda e: e.tensor_copy(out=st_.t[0:8, :], in_=pTr.t[0:8, :]), reads=[pTr.k], writes=[st_.k])

    def a_tiles(h, hook):
        q_, k_, v_, s_, st_ = qa[h % 2], ka[h % 2], vA[h % 2], sz[h % 2], selT[h % 2]
        tiles = []
        for qb in range(4):
            nq = 4 + qb
            nkt = 2 * nq + 2
            po_, pr_ = pO[c_["o"] % 2], pR[c_["o"] % 2]
            c_["o"] += 1
            for kt in range(nkt):
                tiles.append((qb, nq, nkt, kt, po_, pr_))

        def emit_qk(t):
            qb, nq, nkt, kt, po_, pr_ = tiles[t]
            nb = kt // 2
            qs = slice(qb * 256, (qb + 1) * 256)
            ps = pS[t % 2]

            def sm(e):
                e.matmul(ps.t[:, 0:256], lhsT=k_.t[:, kt * 128:(kt + 1) * 128], rhs=q_.t[:, qs], start=True, stop=False)
                if nb < nq:
                    return e.matmul(ps.t[:, 0:256], lhsT=Enb.t[:, nb * 128:(nb + 1) * 128], rhs=st_.t[:, qs],
                                    start=False, stop=True)
                cm_ = cmA0 if kt - 2 * nq == 0 else cmA1
                return e.matmul(ps.t[:, 0:256], lhsT=identb.t[:], rhs=cm_.t[:], start=False, stop=True)
            sch.op("pe", sm, reads=[q_.k, k_.k, Enb.k, st_.k, identb.k, cmA0.k, cmA1.k], writes=[ps.k])

        emit_qk(0)
        for t in range(len(tiles)):
            qb, nq, nkt, kt, po_, pr_ = tiles[t]
            qs = slice(qb * 256, (qb + 1) * 256)
            if t + 1 < len(tiles):
                emit_qk(t + 1)
            ps = pS[t % 2]
            p_ = pt[c_["pt"] % 3]
            c_["pt"] += 1
            bo = coff_biasA(cfg, h, qb, kt)
            sch.op("act", lambda e, ps=ps, p_=p_, bo=bo: e.activation(out=p_.t[:, 0:256], in_=ps.t[:, 0:256], func=AF.Exp,
                                                                      bias=cv("biasA", bo, bo + 1), scale=scaleA),
                   reads=[ps.k, cst.k], writes=[p_.k])

            def pv(e, p_=p_, kt=kt, po_=po_, pr_=pr_, nkt=nkt):
                e.matmul(po_.t[:, 0:256], lhsT=v_.t[:, kt, :], rhs=p_.t[:, 0:256], start=(kt == 0), stop=(kt == nkt - 1))
                return e.matmul(pr_.t[:, 0:256], lhsT=onesb.t[:], rhs=p_.t[:, 0:256], start=(kt == 0),
                                stop=(kt == nkt - 1))
            sch.op("pe", pv, reads=[p_.k, v_.k, onesb.k], writes=[po_.k, pr_.k])
            if kt == nkt - 1:
                ri, tt = rinv[qb % 2], t1[qb % 2]
                sch.op("dve", lambda e, ri=ri, pr_=pr_: e.reciprocal(out=ri.t[:, 0:256], in_=pr_.t[:, 0:256]),
                       reads=[pr_.k], writes=[ri.k])
                sch.op("dve", lambda e, ri=ri, tt=tt, po_=po_: e.tensor_tensor(out=tt.t[:, 0:256], in0=po_.t[:, 0:256],
                                                                              in1=ri.t[:, 0:256], op=ALU.mult),
                       reads=[po_.k, ri.k], writes=[tt.k])
                sch.op("dve", lambda e, tt=tt, qs=qs: e.tensor_tensor(out=yT.t[:, h, qs], in0=tt.t[:, 0:256],
                                                                     in1=s_.t[:, qs], op=ALU.mult),
                       reads=[tt.k, s_.k], writes=[yT.k])
                if qb == 0:
                    hook()

    a_part1(0)
    a_part2(0)
    for h in range(cfg.AH):
        def hook(h=h):
            if h + 1 < cfg.AH:
                a_part2(h + 1)
        if h + 1 < cfg.AH:
            a_part1(h + 1)
        a_tiles(h, hook)
    sch.barrier()
    esb.close()

    esb = ExitStack()
    qB = [alloc(esb, f"b_q{i}", [128, 2, T], BF16) for i in range(2)]
    kB = [alloc(esb, f"b_k{i}", [128, 2, 2048], BF16) for i in range(1)]
    vB = [alloc(esb, f"b_v{i}", [128, 16, 256], BF16) for i in range(1)]
    zB = [alloc(esb, f"b_z{i}", [128, 2, T], BF16) for i in range(2)]
    szB = [alloc(esb, f"b_sz{i}", [128, 2, T], BF16) for i in range(2)]
    ones32 = alloc(esb, "b_ones32", [128, 128], F32)
    dd = [alloc(esb, f"b_dd{i}", [128, 128], F32) for i in range(2)]
    sq = [alloc(esb, f"b_sq{i}", [128, 128], F32) for i in range(2)]
    rs = alloc(esb, "b_rs", [128, 128], F32)
    sch.op("dve", lambda e: e.tensor_copy(out=ones32.t[:], in_=cv("ones")), reads=[cst.k], writes=[ones32.k])
    for h in range(cfg.BH):
        q_, k_, v_, z_, s_ = qB[h % 2], kB[0], vB[0], zB[h % 2], szB[h % 2]
        r0 = h * 256
        sch.dma("sp", q_.t[:], pT[d["PQB"] + r0:d["PQB"] + r0 + 256, :].rearrange("(m p) t -> p m t", p=128), q_,
                reads=RD, writes=[q_.k])
        sch.dma("sp", k_.t[:, :, 0:T], d["kva"][1][r0:r0 + 256, :].rearrange("(m p) t -> p m t", p=128), k_,
                reads=RKA, writes=[k_.k])
        sch.dma("sp", k_.t[:, :, T:2 * T], d["kvo"][1][r0:r0 + 256, :].rearrange("(m p) t -> p m t", p=128), k_,
                reads=RKV, multi=[k_.k])
        with ncd():
            sch.dma("sp", v_.t[:, 0:8, :], d["kva"][3][0:T, r0:r0 + 256].rearrange("(kt p) c -> p kt c", p=128), v_,
                    reads=RKA, writes=[v_.k])
            sch.dma("sp", v_.t[:, 8:16, :], d["kvo"][3][:, r0:r0 + 256].rearrange("(kt p) c -> p kt c", p=128), v_,
                    reads=RKV, multi=[v_.k])
        sch.dma("sp", z_.t[:], pT[d["PZB"] + r0:d["PZB"] + r0 + 256, :].rearrange("(m p) t -> p m t", p=128), z_,
                reads=RD, writes=[z_.k])
        sch.op("act", lambda e, z_=z_, s_=s_: e.activation(out=s_.t[:], in_=z_.t[:], func=AF.Silu),
               reads=[z_.k], writes=[s_.k])
        tiles = []
        for qt in range(8):
            nkt = 8 + qt + 1
            po_, pr_ = pO[c_["o"] % 2], pR[c_["o"] % 2]
            c_["o"] += 1
            for kt in range(nkt):
                tiles.append((qt, nkt, kt, po_, pr_))

        def emit_qk(t, q_=q_, k_=k_, tiles=tiles):
            qt, nkt, kt, po_, pr_ = tiles[t]
            qs = slice(qt * 128, (qt + 1) * 128)
            ps = pS[t % 2]
            diag = (kt == nkt - 1)

            def sm(e):
                ins = None
                for m in range(2):
                    ins = e.matmul(ps.t[:, m * 128:(m + 1) * 128], lhsT=k_.t[:, m, kt * 128:(kt + 1) * 128],
                                   rhs=q_.t[:, m, qs], start=True, stop=not diag)
                    if diag:
                        ins = e.matmul(ps.t[:, m * 128:(m + 1) * 128], lhsT=identb.t[:], rhs=cmB.t[:, 0:128],
                                       start=False, stop=True)
                return ins
            sch.op("pe", sm, reads=[q_.k, k_.k, identb.k, cmB.k], writes=[ps.k])

        def ep1(qt, po_, pr_):
            ri, tt = rinv[qt % 2], t1[qt % 2]
            sch.op("dve", lambda e, ri=ri, pr_=pr_: e.reciprocal(out=ri.t[:, 0:256], in_=pr_.t[:, 0:256]),
                   reads=[pr_.k], writes=[ri.k])
            for c2 in range(2):
                sch.op("dve", lambda e, ri=ri, tt=tt, po_=po_, c2=c2: e.tensor_tensor(
                    out=tt.t[:, c2 * 256:(c2 + 1) * 256], in0=po_.t[:, c2 * 256:(c2 + 1) * 256], in1=ri.t[:, 0:256],
                    op=ALU.mult), reads=[po_.k, ri.k], writes=[tt.k])
                sch.op("dve", lambda e, tt=tt, c2=c2: e.scalar_tensor_tensor(
                    out=dd[c2].t[:], in0=tt.t[:, c2 * 256 + 128:c2 * 256 + 256], scalar=neglam,
                    in1=tt.t[:, c2 * 256:c2 * 256 + 128], op0=ALU.mult, op1=ALU.add),
                    reads=[tt.k, lsc.k], writes=[dd[c2].k])
                sch.op("dve", lambda e, c2=c2: e.tensor_tensor(out=sq[c2].t[:], in0=dd[c2].t[:], in1=dd[c2].t[:], op=ALU.mult),
                       reads=[dd[c2].k], writes=[sq[c2].k])

        def ep2(qt, s_=s_, h=h):
            qs = slice(qt * 128, (qt + 1) * 128)

            def msm(e):
                e.matmul(pG.t[:, 0:128], lhsT=ones32.t[:], rhs=sq[0].t[:], start=True, stop=False)
                return e.matmul(pG.t[:, 0:128], lhsT=ones32.t[:], rhs=sq[1].t[:], start=False, stop=True)
            sch.op("pe", msm, reads=[ones32.k, sq[0].k, sq[1].k], writes=[pG.k])
            sch.op("dve", lambda e: e.tensor_scalar(out=rs.t[:], in0=pG.t[:, 0:128], scalar1=1.0 / 256, scalar2=RMS_EPS,
                                                    op0=ALU.mult, op1=ALU.add), reads=[pG.k], writes=[rs.k])

        def ep2b(qt, s_=s_, h=h):
            qs = slice(qt * 128, (qt + 1) * 128)
            sch.op("act", lambda e: e.activation(out=rs.t[:], in_=rs.t[:], func=AF.Ln), reads=[rs.k], writes=[rs.k])
            sch.op("act", lambda e: e.activation(out=rs.t[:], in_=rs.t[:], func=AF.Exp, scale=-0.5), reads=[rs.k], writes=[rs.k])
            for c2 in range(2):
                sch.op("dve", lambda e, c2=c2: e.scalar_tensor_tensor(out=dd[c2].t[:], in0=dd[c2].t[:], scalar=subg.t[:, c2:c2 + 1],
                                                                     in1=rs.t[:], op0=ALU.mult, op1=ALU.mult),
                       reads=[dd[c2].k, subg.k, rs.k], writes=[dd[c2].k])
                sch.op("dve", lambda e, c2=c2, s_=s_, qs=qs, h=h: e.tensor_tensor(
                    out=yT.t[:, BC + h * 2 + c2, qs], in0=dd[c2].t[:], in1=s_.t[:, c2, qs], op=ALU.mult),
                    reads=[dd[c2].k, s_.k], writes=[yT.k])

        emit_qk(0)
        pend = None
        for t in range(len(tiles)):
            qt, nkt, kt, po_, pr_ = tiles[t]
            if t + 1 < len(tiles):
                emit_qk(t + 1)
            if pend is not None and kt == 7:
                ep2(pend)
            if pend is not None and kt == 9:
                ep2b(pend)
                pend = None
            ps = pS[t % 2]
            p_ = pt[c_["pt"] % 3]
            c_["pt"] += 1
            bo = (h * 8 + qt) * 16 + kt
            sch.op("act", lambda e, ps=ps, p_=p_, bo=bo: e.activation(out=p_.t[:, 0:256], in_=ps.t[:, 0:256], func=AF.Exp,
                                                                      bias=cv("biasB", bo, bo + 1), scale=scaleA),
                   reads=[ps.k, cst.k], writes=[p_.k])

            def pv(e, p_=p_, kt=kt, v_=v_, po_=po_, pr_=pr_, nkt=nkt):
                for c2 in range(2):
                    e.matmul(po_.t[:, c2 * 256:(c2 + 1) * 256], lhsT=v_.t[:, kt, c2 * 128:(c2 + 1) * 128],
                             rhs=p_.t[:, 0:256], start=(kt == 0), stop=(kt == nkt - 1))
                return e.matmul(pr_.t[:, 0:256], lhsT=onesb.t[:], rhs=p_.t[:, 0:256], start=(kt == 0),
                                stop=(kt == nkt - 1))
            sch.op("pe", pv, reads=[p_.k, v_.k, onesb.k], writes=[po_.k, pr_.k])
            if kt == nkt - 1:
                ep1(qt, po_, pr_)
                pend = qt
        ep2(pend)
        ep2b(pend)
    sch.barrier()
    esb.close()
    esb = ExitStack()
    scaleM = 256.0 ** -0.5
    qM = [alloc(esb, f"mm_q{i}", [128, 2, T], BF16) for i in range(2)]
    zM = [alloc(esb, f"mm_z{i}", [128, 2, T], BF16) for i in range(2)]
    szM = [alloc(esb, f"mm_sz{i}", [128, 2, T], BF16) for i in range(2)]
    kM = [alloc(esb, f"mm_k{i}", [128, 2, 256], BF16) for i in range(2)]
    vM = [alloc(esb, f"mm_v{i}", [128, 2, 256], BF16) for i in range(2)]
    for h in range(cfg.MH):
        q_, z_, s_, k_, v_ = qM[h % 2], zM[h % 2], szM[h % 2], kM[h % 2], vM[h % 2]
        r0 = h * 256
        sch.dma("sp", q_.t[:], pT[d["PQM"] + r0:d["PQM"] + r0 + 256, :].rearrange("(m p) t -> p m t", p=128), q_,
                reads=RD, writes=[q_.k])
        sch.dma("sp", z_.t[:], pT[d["PZM"] + r0:d["PZM"] + r0 + 256, :].rearrange("(m p) t -> p m t", p=128), z_,
                reads=RD, writes=[z_.k])
        sch.dma("sp", k_.t[:], d["mkT"][r0:r0 + 256, :].rearrange("(m p) t -> p m t", p=128), k_, reads=RD, writes=[k_.k])
        with ncd():
            sch.dma("sp", v_.t[:], d["mv"][:, r0:r0 + 256].rearrange("(mt p) c -> p mt c", p=128), v_, reads=RD,
                    writes=[v_.k])
        sch.op("act", lambda e, z_=z_, s_=s_: e.activation(out=s_.t[:], in_=z_.t[:], func=AF.Silu),
               reads=[z_.k], writes=[s_.k])
        for hh in range(2):
            qs = slice(hh * 512, (hh + 1) * 512)
            pr_ = pR[hh % 2]
            for mt in range(2):
                ps = pS[c_["s"] % 2]
                c_["s"] += 1
                p_ = pt[c_["pt"] % 3]
                c_["pt"] += 1

                def sm(e, ps=ps, mt=mt, q_=q_, k_=k_, qs=qs):
                    e.matmul(ps.t[:, :], lhsT=k_.t[:, 0, mt * 128:(mt + 1) * 128], rhs=q_.t[:, 0, qs], start=True, stop=False)
                    return e.matmul(ps.t[:, :], lhsT=k_.t[:, 1, mt * 128:(mt + 1) * 128], rhs=q_.t[:, 1, qs], start=False,
                                    stop=True)
                sch.op("pe", sm, reads=[q_.k, k_.k], writes=[ps.k])
                sch.op("act", lambda e, ps=ps, p_=p_: e.activation(out=p_.t[:, :], in_=ps.t[:, :], func=AF.Exp, scale=scaleM),
                       reads=[ps.k], writes=[p_.k])

                def pv(e, p_=p_, mt=mt, v_=v_, pr_=pr_):
                    for c2 in range(2):
                        e.matmul(pO[c2].t[:, :], lhsT=v_.t[:, mt, c2 * 128:(c2 + 1) * 128], rhs=p_.t[:, :], start=(mt == 0),
                                 stop=(mt == 1))
                    return e.matmul(pr_.t[:, :], lhsT=onesb.t[:], rhs=p_.t[:, :], start=(mt == 0), stop=(mt == 1))
                sch.op("pe", pv, reads=[p_.k, v_.k, onesb.k], writes=[pO[0].k, pO[1].k, pr_.k])
            ri = rinv[hh % 2]
            sch.op("dve", lambda e, ri=ri, pr_=pr_: e.reciprocal(out=ri.t[:, :], in_=pr_.t[:, :]), reads=[pr_.k], writes=[ri.k])
            for c2 in range(2):
                tt = t1[c2]
                sch.op("dve", lambda e, ri=ri, tt=tt, c2=c2: e.tensor_tensor(out=tt.t[:, :], in0=pO[c2].t[:, :], in1=ri.t[:, :],
                                                                            op=ALU.mult), reads=[pO[c2].k, ri.k], writes=[tt.k])
                sch.op("dve", lambda e, tt=tt, c2=c2, s_=s_, qs=qs, h=h: e.tensor_tensor(
                    out=yT.t[:, 3 * BC + h * 2 + c2, qs], in0=tt.t[:, :], in1=s_.t[:, c2, qs], op=ALU.mult),
                    reads=[tt.k, s_.k], writes=[yT.k])
    sch.barrier()
    esb.close()
    esb = ExitStack()
    SG = cfg.SG
    GC = 1.5957691216057308
    wT32 = alloc(esb, "c_wT32", [128, SG * 128], F32)
    wTb = alloc(esb, "c_wTb", [128, SG * 128], BF16)
    bsb = alloc(esb, "c_bsb", [128, SG * 128], F32)
    lg = alloc(esb, "c_lg", [128, BW], F32)
    lb = alloc(esb, "c_lb", [128, BW], F32)
    sch.dma("sp", wT32.t[:], d["sguw"][:, :], wT32, writes=[wT32.k])
    sch.dma("sp", bsb.t[:], d["sgubs"][:, :], bsb, writes=[bsb.k])
    sch.dma("sp", lg.t[:], d["sgug"][:, :], lg, writes=[lg.k])
    sch.dma("sp", lb.t[:], d["sgub"][:, :], lb, writes=[lb.k])
    for g in range(SG):
        sch.op("dve", lambda e, g=g: e.tensor_tensor(out=wTb.t[:, g * 128:(g + 1) * 128], in0=wT32.t[:, g * 128:(g + 1) * 128],
                                                     in1=cv("tri"), op=ALU.mult), reads=[wT32.k, cst.k], writes=[wTb.k])
    vcb = [alloc(esb, f"c_vcb{i}", [128, BW], BF16) for i in range(2)]
    ucb = [alloc(esb, f"c_ucb{i}", [128, BW], BF16) for i in range(2)]
    zcb = [alloc(esb, f"c_zcb{i}", [128, BW], BF16) for i in range(2)]
    fa = alloc(esb, "c_fa", [128, BW], F32)
    fb = alloc(esb, "c_fb", [128, BW], F32)
    fc = alloc(esb, "c_fc", [128, BW], F32)
    vn = alloc(esb, "c_vn", [128, BW], BF16)
    cs = alloc(esb, "c_cs", [128, 8], F32)

    def gelu(src, dst, tmp):
        sch.op("dve", lambda e: e.tensor_tensor(out=tmp.t[:], in0=src.t[:], in1=src.t[:], op=ALU.mult),
               reads=[src.k], writes=[tmp.k])
        sch.op("dve", lambda e: e.tensor_scalar(out=tmp.t[:], in0=tmp.t[:], scalar1=0.044715, scalar2=1.0, op0=ALU.mult,
                                                op1=ALU.add), reads=[tmp.k], writes=[tmp.k])
        sch.op("dve", lambda e: e.tensor_tensor(out=tmp.t[:], in0=tmp.t[:], in1=src.t[:], op=ALU.mult),
               reads=[src.k, tmp.k], writes=[tmp.k])
        sch.op("act", lambda e: e.activation(out=tmp.t[:], in_=tmp.t[:], func=AF.Sigmoid, scale=GC),
               reads=[tmp.k], writes=[tmp.k])
        sch.op("dve", lambda e: e.tensor_tensor(out=dst.t[:], in0=tmp.t[:], in1=src.t[:], op=ALU.mult),
               reads=[src.k, tmp.k], writes=[dst.k])

    for ci in range(8):
        v_, u_, z_ = vcb[ci % 2], ucb[ci % 2], zcb[ci % 2]
        ts = slice(ci * 128, (ci + 1) * 128)
        sch.dma("sp", v_.t[:], d["vc"][ci * 128:(ci + 1) * 128, :], v_, reads=RD, writes=[v_.k])
        with ncd():
            sch.dma("sp", u_.t[:].rearrange("p (g t) -> p g t", t=128),
                    pT[d["PUC"]:d["PUC"] + BW, ts].rearrange("(g p) t -> p g t", p=128), u_, reads=RD, writes=[u_.k])
            sch.dma("sp", z_.t[:].rearrange("p (g t) -> p g t", t=128),
                    pT[d["PZC"]:d["PZC"] + BW, ts].rearrange("(g p) t -> p g t", p=128), z_, reads=RD, writes=[z_.k])
        gelu(v_, fa, fb)
        sch.op("dve", lambda e: e.reduce_sum(out=cs.t[:, 0:1], in_=fa.t[:], axis=AX.X), reads=[fa.k], writes=[cs.k])
        sch.op("dve", lambda e: e.tensor_scalar(out=cs.t[:, 1:2], in0=cs.t[:, 0:1], scalar1=-1.0 / BW, scalar2=None,
                                                op0=ALU.mult), reads=[cs.k], writes=[cs.k])
        sch.op("dve", lambda e: e.tensor_scalar(out=fa.t[:], in0=fa.t[:], scalar1=cs.t[:, 1:2], scalar2=None, op0=ALU.add),
               reads=[fa.k, cs.k], writes=[fa.k])
        sch.op("act", lambda e: e.activation(out=vn.t[:], in_=fa.t[:], func=AF.Square, accum_out=cs.t[:, 2:3]),
               reads=[fa.k], writes=[vn.k, cs.k])
        sch.op("dve", lambda e: e.tensor_scalar(out=cs.t[:, 3:4], in0=cs.t[:, 2:3], scalar1=1.0 / BW, scalar2=LN_EPS,
                                                op0=ALU.mult, op1=ALU.add), reads=[cs.k], writes=[cs.k])
        sch.op("act", lambda e: e.activation(out=cs.t[:, 4:5], in_=cs.t[:, 3:4], func=AF.Sqrt), reads=[cs.k], writes=[cs.k])
        sch.op("dve", lambda e: e.reciprocal(out=cs.t[:, 5:6], in_=cs.t[:, 4:5]), reads=[cs.k], writes=[cs.k])
        sch.op("dve", lambda e: e.scalar_tensor_tensor(out=fa.t[:], in0=fa.t[:], scalar=cs.t[:, 5:6], in1=lg.t[:],
                                                       op0=ALU.mult, op1=ALU.mult), reads=[fa.k, cs.k, lg.k], writes=[fa.k])
        sch.op("dve", lambda e: e.tensor_tensor(out=vn.t[:], in0=fa.t[:], in1=lb.t[:], op=ALU.add),
               reads=[fa.k, lb.k], writes=[vn.k])
        pm = [pO[0], pO[1]]

        def mix(e):
            ins = None
            for g in range(SG):
                ins = e.matmul(pm[g // 4].t[:, (g % 4) * 128:(g % 4 + 1) * 128], lhsT=vn.t[:, g * 128:(g + 1) * 128],
                               rhs=wTb.t[:, g * 128:(g + 1) * 128], start=True, stop=True)
            return ins
        sch.op("pe", mix, reads=[vn.k, wTb.k], writes=[pO[0].k, pO[1].k])
        for g4 in range((SG + 3) // 4):
            n = min(4, SG - g4 * 4) * 128
            sch.op("dve", lambda e, g4=g4, n=n: e.tensor_tensor(out=fc.t[:, g4 * 512:g4 * 512 + n], in0=pm[g4].t[:, 0:n],
                                                              in1=bsb.t[:, g4 * 512:g4 * 512 + n], op=ALU.add),
                   reads=[pm[g4].k, bsb.k], writes=[fc.k])
        gelu(u_, fa, fb)
        sch.op("dve", lambda e: e.tensor_tensor(out=fc.t[:], in0=fc.t[:], in1=fa.t[:], op=ALU.mult), reads=[fc.k, fa.k],
               writes=[fc.k])
        sch.op("act", lambda e, z_=z_: e.activation(out=fb.t[:], in_=z_.t[:], func=AF.Silu), reads=[z_.k], writes=[fb.k])
        sch.op("dve", lambda e, ts=ts: e.tensor_tensor(out=yT.t[:, 2 * BC:3 * BC, ts],
                                                       in0=fc.t[:].rearrange("p (g t) -> p g t", t=128),
                                                       in1=fb.t[:].rearrange("p (g t) -> p g t", t=128), op=ALU.mult),
               reads=[fc.k, fb.k], writes=[yT.k])
    sch.barrier()
    esb.close()


def coff_biasA(cfg, h, qb, kt):
    return (h * 4 + qb) * 16 + kt


_NC_CACHE = {}


def _get_nc(cfg, layer_ids):
    key = (cfg.D, cfg.ncores, tuple(layer_ids))
    if key not in _NC_CACHE:
        _NC_CACHE[key] = build_program(cfg, list(layer_ids))
    return _NC_CACHE[key]


def _bc(v, n=128):
    v = np.asarray(v, np.float32).reshape(1, -1)
    return np.ascontiguousarray(np.broadcast_to(v, (n, v.shape[1])))


def run_layers(cfg, layer_ids, x_full, inp):
    T, D, BW = cfg.T, cfg.D, cfg.BW
    nc = _get_nc(cfg, layer_ids)
    f = lambda a: np.ascontiguousarray(np.asarray(a, np.float32))
    shared = {"fgvec": _bc(inp["final_g"])}
    for l in layer_ids:
        s = f"_{l}"
        shared.update({
            "w_in" + s: f(inp["w_in"][l]),
            "w_mkv" + s: f(inp["w_mem_kv"][l]),
            "w_br" + s: f(np.asarray(inp["w_branch"][l]).reshape(4 * BW, D)),
            "w_out" + s: f(inp["w_out"][l]),
            "gvec" + s: _bc(inp["norm_g"][l]),
            "mgvec" + s: _bc(inp["mem_norm_g"][l]),
            "lamv" + s: np.ascontiguousarray(np.concatenate(
                [_bc(inp[k][l]) for k in ("diff_lam_q1", "diff_lam_k1", "diff_lam_q2", "diff_lam_k2")], 1)),
            "subg" + s: f(np.asarray(inp["diff_subln_g"][l]).reshape(2, 128).T),
            "sgug" + s: _bc(inp["sgu_ln_g"][l]),
            "sgub" + s: _bc(inp["sgu_ln_b"][l]),
            "sguwT" + s: f(np.transpose(np.asarray(inp["sgu_w"][l]), (2, 0, 1)).reshape(128, cfg.SG * 128)),
            "sgubs" + s: _bc(np.asarray(inp["sgu_b"][l]).reshape(-1)),
        })
    csts = [build_consts(cfg, 0), build_consts(cfg, 1)]
    in_maps = []
    for c in range(cfg.ncores):
        b, hf = c // 2, c % 2
        m = dict(shared)
        m["xo"] = f(x_full[b, hf * T:(hf + 1) * T])
        m["mem"] = f(inp["mem"][b])
        m["cst"] = csts[hf]
        in_maps.append(m)
    res = run_bass_kernel_spmd(nc, in_maps, core_ids=list(range(cfg.ncores)))
    out = np.empty_like(np.asarray(x_full, np.float32))
    for c in range(cfg.ncores):
        b, hf = c // 2, c % 2
        out[b, hf * T:(hf + 1) * T] = res.results[c]["out"]
    return out


def kernel(**inputs):
    cfg = Cfg(D=4096, depth=2)
    x = np.asarray(inputs["x"], np.float32)
    return run_layers(cfg, list(range(cfg.depth)), x, inputs)
```

```python
import math
import numpy as np
import concourse.bass as bass
import concourse.mybir as mybir
from concourse.bass_utils import run_bass_kernel_spmd

F32 = mybir.dt.float32
BF16 = mybir.dt.bfloat16
AF = mybir.ActivationFunctionType
ALU = mybir.AluOpType
AX = mybir.AxisListType

RMS_EPS = 1e-6
LN_EPS = 1e-5
NEGB = -30000.0


class Cfg:
    def __init__(self, D=4096, depth=2):
        self.D = D
        self.depth = depth
        self.S = 2048
        self.T = 1024
        self.KC = D // 128
        self.BW = D // 4
        self.BC = self.BW // 128
        self.AH = self.BW // 128
        self.BH = self.BW // 256
        self.SG = self.BW // 128
        self.MH = self.BW // 256
        self.NAL = self.AH + self.BH
        self.WIN = 13 * self.BW + 4 * D
        self.ML = 256
        self.ncores = 8


class Tok:
    __slots__ = ("w", "r", "dsem", "dcnt", "name")

    def __init__(self, name=""):
        self.w = {}
        self.r = {}
        self.dsem = None
        self.dcnt = 0
        self.name = name


class Sched:
    def __init__(self, nc):
        self.nc = nc
        self.engs = {"pe": nc.tensor, "act": nc.scalar, "dve": nc.vector, "pool": nc.gpsimd, "sp": nc.sync}
        self.sem = {}
        self.cnt = {}
        self.seen = {k: {} for k in self.engs}
        self.nsem = 0
        self.all_sems = {}
        self.dtoks = []
        self.free_dsems = []
        for k in self.engs:
            self.new_engine_sem(k)

    def alloc_sem(self, name):
        s = self.nc.alloc_semaphore(name=f"{name}_{self.nsem}")
        self.nsem += 1
        self.all_sems[id(s)] = [s, 0]
        return s

    def new_engine_sem(self, k):
        self.sem[k] = self.alloc_sem("e" + k)
        self.cnt[k] = 0

    def _note(self, sem, val):
        self.all_sems[id(sem)][1] = max(self.all_sems[id(sem)][1], val)

    def _wait(self, ename, deps):
        need = {}
        for d in deps:
            for key, (s, v) in d.items():
                if key not in need or need[key][1] < v:
                    need[key] = (s, v)
        e = self.engs[ename]
        seen = self.seen[ename]
        for key, (s, v) in need.items():
            if seen.get(key, 0) >= v:
                continue
            e.wait_ge(s, v)
            seen[key] = v

    def _deps(self, reads, writes):
        deps = []
        for t in reads:
            deps.append(t.w)
        for t in writes:
            deps.append(t.w)
            deps.append(t.r)
        return deps

    def op(self, ename, fn, reads=(), writes=(), pwrites=()):
        deps = self._deps(reads, writes)
        for t in pwrites:
            deps.append(t.r)
        self._wait(ename, deps)
        ins = fn(self.engs[ename])
        self.cnt[ename] += 1
        s = self.sem[ename]
        ins.then_inc(s, 1)
        v = self.cnt[ename]
        self._note(s, v)
        key = id(s)
        for t in writes:
            t.w = {key: (s, v)}
            t.r = {}
        for t in pwrites:
            t.w[key] = (s, v)
        for t in reads:
            t.r[key] = (s, v)

    def dma(self, qname, out, in_, slot, reads=(), writes=(), multi=()):
        deps = self._deps(reads, writes)
        for t in multi:
            deps.append(t.r)
        self._wait(qname, deps)
        slot = slot.k if hasattr(slot, "k") else slot
        if slot.dsem is None:
            if self.free_dsems:
                slot.dsem, slot.dcnt = self.free_dsems.pop()
            else:
                slot.dsem = self.alloc_sem("d")
                slot.dcnt = 0
            self.dtoks.append(slot)
        slot.dcnt += 16
        s, v = slot.dsem, slot.dcnt
        self.engs[qname].dma_start(out=out, in_=in_).then_inc(s, 16)
        self._note(s, v)
        key = id(s)
        for t in writes:
            t.w = {key: (s, v)}
            t.r = {}
        for t in multi:
            t.w[key] = (s, v)
            t.r = {}
        for t in reads:
            t.r[key] = (s, v)

    def barrier(self):
        allv = {k: (s, v) for k, (s, v) in self.all_sems.items() if v > 0}
        for ename in self.engs:
            self._wait(ename, [allv])
        for t in self.dtoks:
            self.free_dsems.append((t.dsem, t.dcnt))
            t.dsem = None
            t.dcnt = 0
        self.dtoks = []


class WStream:
    def __init__(self, sch, stq, wbf, qk):
        self.sch, self.stq, self.wbf, self.qk = sch, stq, wbf, qk
        self.specs = []
        self.nd = 0
        self.ncast = 0
        self.qc = 0
        self.pend = {}
        self.ce = 0

    def add(self, w_ap, K):
        self.specs.append((w_ap, K))
        return len(self.specs) - 1

    def _dma(self, i):
        w_ap, K = self.specs[i]
        kc = K // 128
        src = w_ap.rearrange("(kc p) c -> p kc c", p=128)
        qs = []
        for q0 in range(0, kc, self.qk):
            n = min(self.qk, kc - q0)
            s = self.stq[self.qc % len(self.stq)]
            self.qc += 1
            self.sch.dma("sp", s.t[:, 0:n, :], src[:, q0:q0 + n, :], s, writes=[s.k])
            qs.append((s, q0, n))
        self.pend[i] = qs

    def _cast(self, i):
        b = self.wbf[i % len(self.wbf)]
        for (s, q0, n) in self.pend.pop(i):
            eng = "dve" if self.ce % 2 == 0 else "act"
            self.ce += 1
            if eng == "dve":
                f = lambda e, s=s, q0=q0, n=n, b=b: e.tensor_copy(out=b.t[:, q0:q0 + n, :], in_=s.t[:, 0:n, :])
            else:
                f = lambda e, s=s, q0=q0, n=n, b=b: e.activation(out=b.t[:, q0:q0 + n, :], in_=s.t[:, 0:n, :],
                                                                func=AF.Copy)
            self.sch.op(eng, f, reads=[s.k], pwrites=[b.k])

    def get(self, i):
        n = len(self.specs)
        while True:
            if self.nd < min(i + 3, n) and (self.nd < 2 or self.ncast >= self.nd - 1):
                self._dma(self.nd)
                self.nd += 1
            elif self.ncast < min(i + 2, n) and self.ncast < self.nd:
                self._cast(self.ncast)
                self.ncast += 1
            else:
                break
        assert self.ncast > i
        return self.wbf[i % len(self.wbf)]


def _alibi_slopes(n):
    return np.exp2(-8.0 * np.arange(1, n + 1, dtype=np.float64) / n)


def const_layout(cfg):
    off = {}
    cur = 0

    def add(name, n):
        nonlocal cur
        off[name] = (cur, n)
        cur += n
    add("ident", 128)
    add("ones", 128)
    add("cm", 128)
    add("cmA0", 256)
    add("cmA1", 256)
    add("cmB", 256)
    add("tri", 128)
    add("En", 8 * 128)
    add("biasA", cfg.AH * 4 * 16)
    add("biasB", cfg.BH * 8 * 16)
    add("pastb", 4 * 8)
    add("past01", 4 * 8)
    add("pastb64", 64)
    add("past0164", 64)
    return off, cur


def build_consts(cfg, half):
    off, n = const_layout(cfg)
    c = np.zeros((128, n), np.float32)
    p = np.arange(128)

    def put(name, arr):
        o, m = off[name]
        c[:, o:o + m] = np.asarray(arr, np.float32).reshape(128, m)
    put("ident", np.eye(128))
    put("ones", np.ones((128, 128)))
    cm = np.where(p[:, None] <= p[None, :], 0.0, NEGB)
    put("cm", cm)
    put("cmA0", np.concatenate([cm, np.zeros((128, 128))], 1))
    put("cmA1", np.concatenate([np.full((128, 128), NEGB), cm], 1))
    put("cmB", np.concatenate([cm, cm], 1))
    put("tri", (p[:, None] <= p[None, :]).astype(np.float32))
    en = np.zeros((128, 8, 128), np.float32)
    for nb in range(8):
        en[nb, nb, :] = 1.0
    put("En", en)
    slopes = _alibi_slopes(cfg.NAL)
    sA = slopes[cfg.BH:]
    sB = slopes[:cfg.BH]
    kvalid = np.zeros(16)
    if half == 0:
        kvalid[:8] = NEGB
    bA = np.zeros((128, cfg.AH, 4, 16))
    for h in range(cfg.AH):
        for qb in range(4):
            ref = 1024 + qb * 256 + 128
            for kt in range(16):
                bA[:, h, qb, kt] = sA[h] * (kt * 128 + p - ref) + kvalid[kt]
    put("biasA", bA)
    bB = np.zeros((128, cfg.BH, 8, 16))
    for h in range(cfg.BH):
        for qt in range(8):
            ref = 1024 + qt * 128 + 64
            for kt in range(16):
                bB[:, h, qt, kt] = sB[h] * (kt * 128 + p - ref) + kvalid[kt]
    put("biasB", bB)
    pb = np.zeros((128, 4, 8))
    p01 = np.zeros((128, 4, 8))
    for qb in range(4):
        nq = 4 + qb
        for nb in range(8):
            ok = (nb < nq) and (half == 1 or nb >= 4)
            pb[:, qb, nb] = 0.0 if ok else -1e30
            p01[:, qb, nb] = 1.0 if ok else 0.0
    put("pastb", pb)
    put("past01", p01)
    put("pastb64", np.repeat(pb, 2, axis=1))
    put("past0164", np.repeat(p01, 2, axis=1))
    return c


class Tile:
    def __init__(self, t, name):
        self.t = t
        self.k = Tok(name)


def build_program(cfg, layer_ids):
    D, T, BW = cfg.D, cfg.T, cfg.BW
    nc = bass.Bass("TRN2", target_bir_lowering=False)
    coff, cn = const_layout(cfg)

    def din(name, shape, dt=F32):
        return nc.dram_tensor(name, list(shape), dt, kind="ExternalInput").ap()

    def dscr(name, shape, dt=BF16):
        return nc.dram_tensor(name, list(shape), dt, kind="Internal").ap()

    A = {}
    A["mem"] = din("mem", [256, D])
    A["cst_d"] = din("cst", [128, cn])
    A["fg_d"] = din("fgvec", [128, D])
    out_d = nc.dram_tensor("out", [T, D], F32, kind="ExternalOutput").ap()
    A["pT"] = dscr("pT", [8 * BW, T])
    A["kTa"] = dscr("kTa", [BW, 2048])
    A["kTb"] = dscr("kTb", [BW, 2048])
    A["va_s"] = dscr("va_s", [2048, BW])
    A["vb_s"] = dscr("vb_s", [2048, BW])
    A["vc_s"] = dscr("vc_s", [T, BW])
    A["mkT"] = dscr("mkT", [BW, 256])
    A["mv_s"] = dscr("mv_s", [256, BW])
    A["mgT"] = dscr("mgT", [D, T])
    A["xscr"] = dscr("xscr", [T, D], F32)
    xo = din("xo", [T, D])
    sch = Sched(nc)
    for idx, l in enumerate(layer_ids):
        last = idx == len(layer_ids) - 1
        final = l == cfg.depth - 1
        sfx = f"_{l}"
        A["w_in"] = din("w_in" + sfx, [D, cfg.WIN])
        A["w_mkv"] = din("w_mkv" + sfx, [D, 2 * BW])
        A["w_br"] = din("w_br" + sfx, [4 * BW, D])
        A["w_out"] = din("w_out" + sfx, [D, D])
        A["gv_d"] = din("gvec" + sfx, [128, D])
        A["mgv_d"] = din("mgvec" + sfx, [128, D])
        A["lam_d"] = din("lamv" + sfx, [128, 4 * 128])
        A["subg_d"] = din("subg" + sfx, [128, 2])
        A["sgug_d"] = din("sgug" + sfx, [128, BW])
        A["sgub_d"] = din("sgub" + sfx, [128, BW])
        A["sguw_d"] = din("sguwT" + sfx, [128, cfg.SG * 128])
        A["sgubs_d"] = din("sgubs" + sfx, [128, cfg.SG * 128])
        A["kv_own"] = [nc.dram_tensor(f"kvown{i}{sfx}", sh, BF16) for i, sh in
                       enumerate([[BW, T], [BW, T], [T, BW], [T, BW]])]
        A["kv_all"] = [nc.dram_tensor(f"kvall{i}{sfx}", [2 * sh[0], sh[1]], BF16) for i, sh in
                       enumerate([[BW, T], [BW, T], [T, BW], [T, BW]])]
        A["xo"] = xo
        if last:
            A["dest"] = out_d
        else:
            x1_t = nc.dram_tensor(f"x1buf{sfx}", [T, D], F32)
            A["dest"] = x1_t.ap()
            xo = x1_t.ap()
        emit_layer(nc, sch, cfg, l, final, A, f"L{l}_", None)
        sch.barrier()
    return nc


def emit_layer(nc, sch, cfg, layer_idx, final, A, pfx, tk_x):
    D, T, KC, BW, BC = cfg.D, cfg.T, cfg.KC, cfg.BW, cfg.BC
    coff, cn = const_layout(cfg)
    lam_init = 0.8 - 0.6 * math.exp(-0.3 * layer_idx)
    xo, mem = A["xo"], A["mem"]
    kvo = [t_.ap() for t_ in A["kv_own"]]
    kva = [t_.ap() for t_ in A["kv_all"]]
    w_in, w_mkv, w_br, w_out = A["w_in"], A["w_mkv"], A["w_br"], A["w_out"]
    gv_d, mgv_d, fg_d, lam_d, subg_d = A["gv_d"], A["mgv_d"], A["fg_d"], A["lam_d"], A["subg_d"]
    sgug_d, sgub_d, sguw_d, sgubs_d, cst_d = A["sgug_d"], A["sgub_d"], A["sguw_d"], A["sgubs_d"], A["cst_d"]
    out_d = A["dest"]
    pT, kTa, kTb, va_s, vb_s, vc_s, mkT, mv_s, mgT = (A[k] for k in ("pT", "kTa", "kTb", "va_s", "vb_s", "vc_s",
                                                                      "mkT", "mv_s", "mgT"))
    PQA, PZA, PQB, PZB, PUC, PZC, PQM, PZM = [i * BW for i in range(8)]
    xout = A["xscr"] if final else out_d
    XRD = [tk_x] if tk_x is not None else []
    tk_scr = Tok("scr")
    tk_kv = Tok("kv")
    tk_kvall = Tok("kvall")
    tk_mg = Tok("mg")
    tk_xout = Tok("xout")

    from contextlib import ExitStack

    def alloc(es, name, shape, dt):
        return Tile(es.enter_context(nc.sbuf_tensor(pfx + "sb_" + name, list(shape), dt)), name)

    def palloc(es, name, shape, dt):
        return Tile(es.enter_context(nc.psum_tensor(pfx + "ps_" + name, list(shape), dt)), name)

    with ExitStack() as es0:
        hT = alloc(es0, "hT", [128, KC, T], BF16)

        with ExitStack() as es:
            xs = [alloc(es, f"xs{i}", [128, D], F32) for i in range(2)]
            xn = [alloc(es, f"xn{i}", [128, D], BF16) for i in range(2)]
            gvt = alloc(es, "gvt", [128, D], F32)
            hmT = alloc(es, "hmT", [128, KC, 256], BF16)
            cid = alloc(es, "cid", [128, 128], F32)
            sch.dma("sp", cid.t[:], cst_d[:, coff["ident"][0]:coff["ident"][0] + 128], cid, writes=[cid.k])
            st = [alloc(es, f"st{i}", [128, 4]) if False else alloc(es, f"st{i}", [128, 4], F32) for i in range(2)]
            QK = min(8, KC)
            stq = [alloc(es, f"stq{i}", [128, QK, 128], F32) for i in range(8)]
            wbf = [alloc(es, f"wbf{i}", [128, KC, 128], BF16) for i in range(3)]
            wsm = WStream(sch, stq, wbf, QK)
            otl = [alloc(es, f"otl{i}", [128, T], BF16) for i in range(3)]
            identb = alloc(es, "identb", [128, 128], BF16)
            ptr = [palloc(es, f"ptr{i}", [128, 1024], BF16) for i in range(2)]
            pacc = [palloc(es, f"pacc{i}", [128, 512], F32) for i in range(4)]
            sch.op("dve", lambda e: e.tensor_copy(out=identb.t[:], in_=cid.t[:]), reads=[cid.k], writes=[identb.k])
            cnt = {"x": 0, "w": 0, "wb": 0, "ot": 0, "pa": 0, "pt": 0}

            def norm_transpose(x_ap, ntok, gsrc, hdst):
                sch.dma("sp", gvt.t[:], gsrc[:, :], gvt, writes=[gvt.k])
                def stage_a(i):
                        a = xs[cnt["x"] % 2]
                        b = xn[cnt["x"] % 2]
                        s4 = st[cnt["x"] % 2]
                        cnt["x"] += 1
                        sch.dma("sp", a.t[:], x_ap(i) if callable(x_ap) else x_ap[i * 128:(i + 1) * 128, :], a, reads=XRD, writes=[a.k])
                        sch.op("act", lambda e: e.activation(out=b.t[:], in_=a.t[:], func=AF.Square,
                                                             accum_out=s4.t[:, 0:1]),
                               reads=[a.k], writes=[b.k, s4.k])
                        sch.op("dve", lambda e: e.tensor_scalar(out=s4.t[:, 1:2], in0=s4.t[:, 0:1], scalar1=1.0 / D,
                                                                scalar2=RMS_EPS, op0=ALU.mult, op1=ALU.add),
                               reads=[s4.k], writes=[s4.k])
                        sch.op("act", lambda e: e.activation(out=s4.t[:, 2:3], in_=s4.t[:, 1:2], func=AF.Sqrt),
                               reads=[s4.k], writes=[s4.k])
                        sch.op("dve", lambda e: e.reciprocal(out=s4.t[:, 3:4], in_=s4.t[:, 2:3]),
                               reads=[s4.k], writes=[s4.k])
                        sch.op("dve", lambda e: e.scalar_tensor_tensor(out=b.t[:], in0=a.t[:], scalar=s4.t[:, 3:4],
                                                                       in1=gvt.t[:], op0=ALU.mult, op1=ALU.mult),
                               reads=[a.k, s4.k, gvt.k], writes=[b.k])

                        return b

                def stage_b(i, b):
                        for g8 in range(KC // 8):
                            ps = ptr[cnt["pt"] % 2]
                            cnt["pt"] += 1

                            def tr(e, ps=ps, g8=g8, b=b):
                                ins = None
                                for j in range(8):
                                    kc = g8 * 8 + j
                                    ins = e.transpose(out=ps.t[:, j * 128:(j + 1) * 128],
                                                      in_=b.t[:, kc * 128:(kc + 1) * 128], identity=identb.t[:])
                                return ins
                            sch.op("pe", tr, reads=[b.k, identb.k], writes=[ps.k])
                            eng = "dve" if g8 % 2 == 0 else "act"
                            if eng == "dve":
                                f = lambda e, ps=ps, g8=g8, i=i: e.tensor_copy(
                                    out=hdst.t[:, g8 * 8:(g8 + 1) * 8, i * 128:(i + 1) * 128],
                                    in_=ps.t[:].rearrange("p (j t) -> p j t", t=128))
                            else:
                                f = lambda e, ps=ps, g8=g8, i=i: e.activation(
                                    out=hdst.t[:, g8 * 8:(g8 + 1) * 8, i * 128:(i + 1) * 128],
                                    in_=ps.t[:].rearrange("p (j t) -> p j t", t=128), func=AF.Copy)
                            sch.op(eng, f, reads=[ps.k], writes=[hdst.k])

                nt_ = ntok // 128
                pend_b = stage_a(0)
                for i in range(nt_):
                    cur = pend_b
                    if i + 1 < nt_:
                        pend_b = stage_a(i + 1)
                    stage_b(i, cur)

            def sweep(src_tile, nkc, ntok, wbase, nchunks, mode, dest, gtok, j0=0):
                for j in range(j0, j0 + nchunks):
                    wb = wsm.get(wbase + j - j0)
                    ot = otl[cnt["ot"] % 3]
                    cnt["ot"] += 1
                    if mode == "fm":
                        nh = max(1, ntok // 512)
                        n = ntok // nh
                        for hh in range(nh):
                            ps = pacc[cnt["pa"] % 4]
                            cnt["pa"] += 1

                            def mm(e, ps=ps, hh=hh, n=n, wb=wb):
                                ins = None
                                for k in range(nkc):
                                    ins = e.matmul(ps.t[:, 0:n], lhsT=wb.t[:, k, :],
                                                   rhs=src_tile.t[:, k, hh * n:(hh + 1) * n],
                                                   start=(k == 0), stop=(k == nkc - 1))
                                return ins
                            sch.op("pe", mm, reads=[wb.k, src_tile.k], writes=[ps.k])
                            eng = "dve" if hh % 2 == 0 else "act"
                            if eng == "dve":
                                f = lambda e, ps=ps, hh=hh, n=n, ot=ot: e.tensor_copy(
                                    out=ot.t[:, hh * n:(hh + 1) * n], in_=ps.t[:, 0:n])
                            else:
                                f = lambda e, ps=ps, hh=hh, n=n, ot=ot: e.activation(
                                    out=ot.t[:, hh * n:(hh + 1) * n], in_=ps.t[:, 0:n], func=AF.Copy)
                            sch.op(eng, f, reads=[ps.k], writes=[ot.k])
                        sch.dma("sp", dest(j), ot.t[:, 0:ntok], ot, reads=[ot.k], multi=[gtok])
                    else:
                        nt = ntok // 128
                        for g4 in range((nt + 3) // 4):
                            ps = pacc[cnt["pa"] % 4]
                            cnt["pa"] += 1
                            m = min(4, nt - g4 * 4)

                            def mm(e, ps=ps, g4=g4, m=m, wb=wb):
                                ins = None
                                for ii in range(m):
                                    i = g4 * 4 + ii
                                    for k in range(nkc):
                                        ins = e.matmul(ps.t[:, ii * 128:(ii + 1) * 128],
                                                       lhsT=src_tile.t[:, k, i * 128:(i + 1) * 128],
                                                       rhs=wb.t[:, k, :], start=(k == 0), stop=(k == nkc - 1))
                                return ins
                            sch.op("pe", mm, reads=[wb.k, src_tile.k], writes=[ps.k])
                            eng = "dve" if g4 % 2 == 0 else "act"
                            if eng == "dve":
                                f = lambda e, ps=ps, g4=g4, m=m, ot=ot: e.tensor_copy(
                                    out=ot.t[:, g4 * 512:g4 * 512 + m * 128], in_=ps.t[:, 0:m * 128])
                            else:
                                f = lambda e, ps=ps, g4=g4, m=m, ot=ot: e.activation(
                                    out=ot.t[:, g4 * 512:g4 * 512 + m * 128], in_=ps.t[:, 0:m * 128], func=AF.Copy)
                            sch.op(eng, f, reads=[ps.k], writes=[ot.k])
                        with nc.allow_non_contiguous_dma(reason="token-major scratch rows"):
                            sch.dma("sp", dest(j).rearrange("(i p) c -> p i c", p=128),
                                    ot.t[:, 0:ntok].rearrange("p (i c) -> p i c", c=128), ot,
                                    reads=[ot.k], multi=[gtok])

            def wcol(w_ap, c0):
                return lambda j: w_ap[:, c0 + j * 128:c0 + (j + 1) * 128]

            fm_blocks = [(0, PQA), (3, PZA), (4, PQB), (7, PZB), (8, PUC), (10, PZC), (11, PQM), (12, PZM)]
            plan = [("norm", mem, 256, mgv_d, hmT),
                    ("norm", xo, T, gv_d, hT),
                    ("sw", T, w_in, 1 * BW, "fm", lambda j: kvo[0][j * 128:(j + 1) * 128, :], tk_kv, hT, 0, BC),
                    ("sw", T, w_in, 5 * BW, "fm", lambda j: kvo[1][j * 128:(j + 1) * 128, :], tk_kv, hT, 0, BC),
                    ("sw", T, w_in, 2 * BW, "tm", lambda j: kvo[2][:, j * 128:(j + 1) * 128], tk_kv, hT, 0, BC),
                    ("sw", T, w_in, 6 * BW, "tm", lambda j: kvo[3][:, j * 128:(j + 1) * 128], tk_kv, hT, 0, BC),
                    ("xchg",)]
            memq = [("sw", 256, w_mkv, 0, "fm", lambda j: mkT[j * 128:(j + 1) * 128, :], tk_scr, hmT, j, 1)
                    for j in range(BC)]
            memq += [("sw", 256, w_mkv, BW, "tm", lambda j: mv_s[:, j * 128:(j + 1) * 128], tk_scr, hmT, j, 1)
                     for j in range(BC)]
            big = []
            for blk, prow in fm_blocks:
                big.append((blk, "fm", lambda j, prow=prow: pT[prow + j * 128:prow + (j + 1) * 128, :]))
            big.append((9, "tm", lambda j: vc_s[:, j * 128:(j + 1) * 128]))
            hb = max(1, BC // 2)
            for blk, mode_, dfn in big:
                for j0_ in range(0, BC, hb):
                    plan.append(("sw", T, w_in, blk * BW, mode_, dfn, tk_scr, hT, j0_, min(hb, BC - j0_)))
                    if memq:
                        plan.append(memq.pop(0))
            plan += memq
            bases = []
            for it in plan:
                if it[0] == "sw":
                    bases.append(len(wsm.specs))
                    for j in range(it[8], it[8] + it[9]):
                        wsm.add(it[2][:, it[3] + j * 128:it[3] + (j + 1) * 128], D)
                else:
                    bases.append(None)
            for it, wbase in zip(plan, bases):
                if it[0] == "norm":
                    norm_transpose(it[1], it[2], it[3], it[4])
                elif it[0] == "xchg":
                    sch._wait("pool", [tk_kv.w])
                    csem = sch.alloc_sem("cc")
                    groups = [[2 * i, 2 * i + 1] for i in range(cfg.ncores // 2)]
                    for i in range(4):
                        cc = nc.gpsimd.collective_compute("AllGather", ALU.bypass, replica_groups=groups,
                                                          ins=[A["kv_own"][i].ap().opt()],
                                                          outs=[A["kv_all"][i].ap().opt()])
                        cc.then_inc(csem)
                    sch._note(csem, 4)
                    tk_kvall.w = {id(csem): (csem, 4)}
                else:
                    sweep(it[7], KC, it[1], wbase, it[9], it[4], it[5], it[6], it[8])
            sch.barrier()

        with ExitStack() as es1:
            yT = alloc(es1, "yT", [128, 4 * BC, T], BF16)
            with ExitStack() as es:
                build_mixers(nc, sch, cfg, es, alloc, palloc, cst_d, coff, cn, yT, tk_scr, lam_init,
                             dict(pT=pT, kvo=kvo, kva=kva, tk_kv=tk_kv, tk_kvall=tk_kvall, vc=vc_s, mkT=mkT, mv=mv_s,
                                  PQA=PQA, PZA=PZA, PQB=PQB, PZB=PZB, PUC=PUC, PZC=PZC, PQM=PQM, PZM=PZM,
                                  lam=lam_d, subg=subg_d, sgug=sgug_d, sgub=sgub_d, sguw=sguw_d, sgubs=sgubs_d))
                sch.barrier()
            with ExitStack() as es:
                QK = min(8, KC)
                stq = [alloc(es, f"gstq{i}", [128, QK, 128], F32) for i in range(8)]
                wbf = [alloc(es, f"gwbf{i}", [128, KC, 128], BF16) for i in range(3)]
                wsm = WStream(sch, stq, wbf, QK)
                sg = [alloc(es, f"sg{i}", [128, T], F32) for i in range(2)]
                acc = [alloc(es, f"acc{i}", [128, T], F32) for i in range(1)]
                tmp = [alloc(es, f"tmp{i}", [128, T], F32) for i in range(1)]
                mo = [alloc(es, f"mo{i}", [128, T], BF16) for i in range(2)]
                pg = [palloc(es, f"pg{i}", [128, 512], F32) for i in range(4)]
                pb = [palloc(es, f"pb{i}", [128, 512], F32) for i in range(4)]
                cnt = {"w": 0, "wb": 0, "pg": 0, "pb": 0, "sg": 0, "tmp": 0}

                specs = []
                for c in range(KC):
                    for b in range(4):
                        wsm.add(w_in[:, 13 * BW + b * D + c * 128:13 * BW + b * D + (c + 1) * 128], D)
                        wsm.add(w_br[b * BW:(b + 1) * BW, c * 128:(c + 1) * 128], BW)
                getw = wsm.get
                for c in range(KC):
                    ac = acc[0]
                    for b in range(4):
                        wg = getw((c * 4 + b) * 2)
                        sgt = sg[cnt["sg"] % 2]
                        cnt["sg"] += 1
                        for hh in range(2):
                            ps = pg[cnt["pg"] % 4]
                            cnt["pg"] += 1

                            def mm(e, ps=ps, hh=hh, wg=wg):
                                ins = None
                                for k in range(KC):
                                    ins = e.matmul(ps.t[:, :], lhsT=wg.t[:, k, :], rhs=hT.t[:, k, hh * 512:(hh + 1) * 512],
                                                   start=(k == 0), stop=(k == KC - 1))
                                return ins
                            sch.op("pe", mm, reads=[wg.k, hT.k], writes=[ps.k])
                            sch.op("act", lambda e, ps=ps, hh=hh, sgt=sgt: e.activation(
                                out=sgt.t[:, hh * 512:(hh + 1) * 512], in_=ps.t[:, :], func=AF.Sigmoid),
                                reads=[ps.k], writes=[sgt.k])
                        wb_ = getw((c * 4 + b) * 2 + 1)
                        for hh in range(2):
                            ps = pb[cnt["pb"] % 4]
                            cnt["pb"] += 1

                            def mm2(e, ps=ps, hh=hh, wb_=wb_, b=b):
                                ins = None
                                for k in range(BC):
                                    ins = e.matmul(ps.t[:, :], lhsT=wb_.t[:, k, :],
                                                   rhs=yT.t[:, b * BC + k, hh * 512:(hh + 1) * 512],
                                                   start=(k == 0), stop=(k == BC - 1))
                                return ins
                            sch.op("pe", mm2, reads=[wb_.k, yT.k], writes=[ps.k])
                            sl = slice(hh * 512, (hh + 1) * 512)
                            if b == 0:
                                sch.op("dve", lambda e, ps=ps, sl=sl, sgt=sgt, ac=ac: e.tensor_tensor(
                                    out=ac.t[:, sl], in0=ps.t[:, :], in1=sgt.t[:, sl], op=ALU.mult),
                                    reads=[ps.k, sgt.k], writes=[ac.k])
                            else:
                                tm_ = tmp[0]
                                cnt["tmp"] += 1
                                sch.op("dve", lambda e, ps=ps, sl=sl, sgt=sgt, tm_=tm_: e.tensor_tensor(
                                    out=tm_.t[:, sl], in0=ps.t[:, :], in1=sgt.t[:, sl], op=ALU.mult),
                                    reads=[ps.k, sgt.k], writes=[tm_.k])
                                if b < 3:
                                    sch.op("pool", lambda e, sl=sl, tm_=tm_, ac=ac: e.tensor_tensor(
                                        out=ac.t[:, sl], in0=ac.t[:, sl], in1=tm_.t[:, sl], op=ALU.add),
                                        reads=[tm_.k, ac.k], writes=[ac.k])
                                else:
                                    m_ = mo[c % 2]
                                    sch.op("pool", lambda e, sl=sl, tm_=tm_, ac=ac, m_=m_: e.tensor_tensor(
                                        out=m_.t[:, sl], in0=ac.t[:, sl], in1=tm_.t[:, sl], op=ALU.add),
                                        reads=[tm_.k, ac.k], writes=[m_.k])
                    m_ = mo[c % 2]
                    sch.dma("sp", mgT[c * 128:(c + 1) * 128, :], m_.t[:, :], m_, reads=[m_.k], multi=[tk_mg])
                sch.barrier()
            with ExitStack() as es:
                QK = min(8, KC)
                stq = [alloc(es, f"ostq{i}", [128, QK, 128], F32) for i in range(8)]
                wbf = [alloc(es, f"owbf{i}", [128, KC, 128], BF16) for i in range(3)]
                wsm = WStream(sch, stq, wbf, QK)
                for j in range(KC):
                    wsm.add(w_out[:, j * 128:(j + 1) * 128], D)
                xt = [alloc(es, f"xt{i}", [128, 8, 128], F32) for i in range(2)]
                ob = [alloc(es, f"ob{i}", [128, 8, 128], F32) for i in range(2)]
                po = [palloc(es, f"po{i}", [128, 512], F32) for i in range(4)]
                for q in range(4):
                    kq = KC // 4
                    sch.dma("sp", hT.t[:, q * kq:(q + 1) * kq, :],
                            mgT[q * kq * 128:(q + 1) * kq * 128, :].rearrange("(kc p) t -> p kc t", p=128),
                            hT, reads=[tk_mg], multi=[hT.k])
                cp = 0

                for j in range(KC):
                    b = wsm.get(j)
                    x_ = xt[j % 2]
                    o_ = ob[j % 2]
                    with nc.allow_non_contiguous_dma(reason="column block of x"):
                        sch.dma("sp", x_.t[:], xo[:, j * 128:(j + 1) * 128].rearrange("(i p) c -> p i c", p=128),
                                x_, reads=XRD, writes=[x_.k])
                    for g4 in range(2):
                        ps = po[cp % 4]
                        cp += 1

                        def mm(e, ps=ps, g4=g4, b=b):
                            ins = None
                            for ii in range(4):
                                i = g4 * 4 + ii
                                for k in range(KC):
                                    ins = e.matmul(ps.t[:, ii * 128:(ii + 1) * 128],
                                                   lhsT=hT.t[:, k, i * 128:(i + 1) * 128], rhs=b.t[:, k, :],
                                                   start=(k == 0), stop=(k == KC - 1))
                            return ins
                        sch.op("pe", mm, reads=[b.k, hT.k], writes=[ps.k])
                        sch.op("dve", lambda e, ps=ps, g4=g4, x_=x_, o_=o_: e.tensor_tensor(
                            out=o_.t[:, g4 * 4:(g4 + 1) * 4, :], in0=ps.t[:].rearrange("p (i c) -> p i c", c=128),
                            in1=x_.t[:, g4 * 4:(g4 + 1) * 4, :], op=ALU.add),
                            reads=[ps.k, x_.k], writes=[o_.k])
                    with nc.allow_non_contiguous_dma(reason="column block of out"):
                        sch.dma("sp", xout[:, j * 128:(j + 1) * 128].rearrange("(i p) c -> p i c", p=128), o_.t[:],
                                o_, reads=[o_.k], multi=[tk_xout])
                sch.barrier()
            if final:
                with ExitStack() as es:
                    fx = [alloc(es, f"fx{i}", [128, D], F32) for i in range(2)]
                    fgt = alloc(es, "fgt", [128, D], F32)
                    fj = alloc(es, "fj", [128, D], BF16)
                    fs = [alloc(es, f"fs{i}", [128, 4], F32) for i in range(2)]
                    sch.dma("sp", fgt.t[:], fg_d[:, :], fgt, writes=[fgt.k])
                    for i in range(T // 128):
                        a = fx[i % 2]
                        o_ = a
                        s4 = fs[i % 2]
                        sch.dma("sp", a.t[:], xout[i * 128:(i + 1) * 128, :], a, reads=[tk_xout], writes=[a.k])
                        sch.op("act", lambda e, a=a, s4=s4: e.activation(out=fj.t[:], in_=a.t[:], func=AF.Square,
                                                                         accum_out=s4.t[:, 0:1]),
                               reads=[a.k], writes=[fj.k, s4.k])
                        sch.op("dve", lambda e, s4=s4: e.tensor_scalar(out=s4.t[:, 1:2], in0=s4.t[:, 0:1],
                                                                       scalar1=1.0 / D, scalar2=RMS_EPS,
                                                                       op0=ALU.mult, op1=ALU.add),
                               reads=[s4.k], writes=[s4.k])
                        sch.op("act", lambda e, s4=s4: e.activation(out=s4.t[:, 2:3], in_=s4.t[:, 1:2], func=AF.Sqrt),
                               reads=[s4.k], writes=[s4.k])
                        sch.op("dve", lambda e, s4=s4: e.reciprocal(out=s4.t[:, 3:4], in_=s4.t[:, 2:3]),
                               reads=[s4.k], writes=[s4.k])
                        sch.op("dve", lambda e, a=a, s4=s4, o_=o_: e.scalar_tensor_tensor(
                            out=o_.t[:], in0=a.t[:], scalar=s4.t[:, 3:4], in1=fgt.t[:], op0=ALU.mult, op1=ALU.mult),
                            reads=[a.k, s4.k, fgt.k], writes=[o_.k])
                        sch.dma("sp", out_d[i * 128:(i + 1) * 128, :], o_.t[:], o_, reads=[o_.k])
                    sch.barrier()


def build_mixers(nc, sch, cfg, es, alloc, palloc, cst_d, coff, cn, yT, tk_scr, lam_init, d):
    from contextlib import ExitStack
    cst = alloc(es, "cst", [128, cn], F32)
    sch.dma("sp", cst.t[:], cst_d[:, :], cst, writes=[cst.k])

    def cv(name, a=0, b=None):
        o, m = coff[name]
        b = m if b is None else b
        return cst.t[:, o + a:o + b]
    T, BW, BC = cfg.T, cfg.BW, cfg.BC
    pT = d["pT"]
    RD = [tk_scr]
    RKV = [d["tk_kv"]]
    RKA = [d["tk_kvall"]]

    def ncd():
        return nc.allow_non_contiguous_dma(reason="scratch layout")

    identb = alloc(es, "m_identb", [128, 128], BF16)
    onesb = alloc(es, "m_onesb", [128, 128], BF16)
    cmA0 = alloc(es, "m_cmA0", [128, 256], BF16)
    cmA1 = alloc(es, "m_cmA1", [128, 256], BF16)
    cmB = alloc(es, "m_cmB", [128, 256], BF16)
    Enb = alloc(es, "m_En", [128, 8 * 128], BF16)
    for tl, nm in ((identb, "ident"), (onesb, "ones"), (cmA0, "cmA0"), (cmA1, "cmA1"), (cmB, "cmB"), (Enb, "En")):
        sch.op("dve", lambda e, tl=tl, nm=nm: e.tensor_copy(out=tl.t[:], in_=cv(nm)), reads=[cst.k], writes=[tl.k])
    lamt = alloc(es, "m_lamt", [128, 4 * 128], F32)
    lsc = alloc(es, "m_lsc", [128, 8], F32)
    ljunk = alloc(es, "m_ljunk", [128, 128], F32)
    subg = alloc(es, "m_subg", [128, 2], F32)
    sch.dma("sp", lamt.t[:], d["lam"][:, :], lamt, writes=[lamt.k])
    sch.dma("sp", subg.t[:], d["subg"][:, :], subg, writes=[subg.k])
    for i in range(2):
        sch.op("dve", lambda e, i=i: e.tensor_tensor(out=ljunk.t[:], in0=lamt.t[:, (2 * i) * 128:(2 * i + 1) * 128],
                                                     in1=lamt.t[:, (2 * i + 1) * 128:(2 * i + 2) * 128], op=ALU.mult),
               reads=[lamt.k], writes=[ljunk.k])
        sch.op("dve", lambda e, i=i: e.reduce_sum(out=lsc.t[:, i:i + 1], in_=ljunk.t[:], axis=AX.X),
               reads=[ljunk.k], writes=[lsc.k])
    sch.op("act", lambda e: e.activation(out=lsc.t[:, 2:4], in_=lsc.t[:, 0:2], func=AF.Exp), reads=[lsc.k], writes=[lsc.k])
    sch.op("dve", lambda e: e.scalar_tensor_tensor(out=lsc.t[:, 4:5], in0=lsc.t[:, 3:4], scalar=-lam_init,
                                                   in1=lsc.t[:, 2:3], op0=ALU.add, op1=ALU.subtract),
           reads=[lsc.k], writes=[lsc.k])
    sch.op("dve", lambda e: e.tensor_scalar(out=subg.t[:], in0=subg.t[:], scalar1=(1.0 - lam_init), scalar2=None,
                                            op0=ALU.mult), reads=[subg.k], writes=[subg.k])
    neglam = lsc.t[:, 4:5]

    pS = [palloc(es, f"pS{i}", [128, 512], F32) for i in range(2)]
    pO = [palloc(es, f"pO{i}", [128, 512], F32) for i in range(2)]
    pR = [palloc(es, f"pR{i}", [128, 512], F32) for i in range(2)]
    pG = palloc(es, "pG", [128, 512], F32)
    pTr = palloc(es, "pTr", [128, 1024], BF16)

    pt = [alloc(es, f"m_pt{i}", [128, 512], BF16) for i in range(3)]
    rinv = [alloc(es, f"m_rinv{i}", [128, 512], F32) for i in range(2)]
    t1 = [alloc(es, f"m_t1{i}", [128, 512], F32) for i in range(2)]
    c_ = {"pt": 0, "s": 0, "o": 0, "ld": 0}

    esb = ExitStack()
    scaleA = 128.0 ** -0.5
    qa = [alloc(esb, f"a_q{i}", [128, T], BF16) for i in range(2)]
    ka = [alloc(esb, f"a_k{i}", [128, 2048], BF16) for i in range(2)]
    vA = [alloc(esb, f"a_v{i}", [128, 16, 128], BF16) for i in range(2)]
    za = [alloc(esb, f"a_z{i}", [128, T], BF16) for i in range(2)]
    sz = [alloc(esb, f"a_sz{i}", [128, T], BF16) for i in range(2)]
    km32 = alloc(esb, "a_km32", [128, 8], F32)
    kmhl = [alloc(esb, f"a_kmhl{i}", [128, 16], BF16) for i in range(2)]
    kmh32 = alloc(esb, "a_kmh32", [128, 8], F32)
    g64 = alloc(esb, "a_g64", [128, 64], F32)
    top64 = alloc(esb, "a_top64", [128, 64], F32)
    sel64 = alloc(esb, "a_sel64", [128, 64], F32)
    selq = alloc(esb, "a_selq", [128, 64], BF16)
    selT = [alloc(esb, f"a_selT{i}", [128, T], BF16) for i in range(2)]
    for st0 in selT:
        sch.op("dve", lambda e, st0=st0: e.memset(st0.t[:], 0.0), writes=[st0.k])

    def a_part1(h):
        q_, k_, v_, z_, s_ = qa[h % 2], ka[h % 2], vA[h % 2], za[h % 2], sz[h % 2]
        km_ = kmhl[h % 2]
        r0 = h * 128
        sch.dma("sp", q_.t[:], pT[d["PQA"] + r0:d["PQA"] + r0 + 128, :], q_, reads=RD, writes=[q_.k])
        sch.dma("sp", k_.t[:, 0:T], d["kva"][0][r0:r0 + 128, :], k_, reads=RKA, writes=[k_.k])
        sch.dma("sp", k_.t[:, T:2 * T], d["kvo"][0][r0:r0 + 128, :], k_, reads=RKV, multi=[k_.k])
        with ncd():
            sch.dma("sp", v_.t[:, 0:8, :], d["kva"][2][0:T, r0:r0 + 128].rearrange("(kt p) c -> p kt c", p=128), v_,
                    reads=RKA, writes=[v_.k])
            sch.dma("sp", v_.t[:, 8:16, :], d["kvo"][2][:, r0:r0 + 128].rearrange("(kt p) c -> p kt c", p=128), v_,
                    reads=RKV, multi=[v_.k])
        sch.dma("sp", z_.t[:], pT[d["PZA"] + r0:d["PZA"] + r0 + 128, :], z_, reads=RD, writes=[z_.k])
        sch.op("act", lambda e: e.activation(out=s_.t[:], in_=z_.t[:], func=AF.Silu), reads=[z_.k], writes=[s_.k])
        sch.op("dve", lambda e: e.tensor_reduce(out=km32.t[:], in_=k_.t[:].rearrange("p (n s) -> p n s", s=256),
                                                axis=AX.X, op=ALU.add), reads=[k_.k], writes=[km32.k])
        sch.op("dve", lambda e: e.tensor_copy(out=km_.t[:, 0:8], in_=km32.t[:]), reads=[km32.k], writes=[km_.k])
        sch.op("dve", lambda e: e.tensor_copy(out=kmh32.t[:], in_=km_.t[:, 0:8]), reads=[km_.k], writes=[kmh32.k])
        sch.op("dve", lambda e: e.tensor_tensor(out=km_.t[:, 8:16], in0=km32.t[:], in1=kmh32.t[:], op=ALU.subtract),
               reads=[km32.k, kmh32.k, km_.k], writes=[km_.k])

    def a_part1b(h):
        q_ = qa[h % 2]
        km_ = kmhl[h % 2]

        def gm(e):
            ins = None
            for qt in range(8):
                e.matmul(pG.t[:, qt * 8:qt * 8 + 8], lhsT=q_.t[:, qt * 128:(qt + 1) * 128], rhs=km_.t[:, 0:8],
                         start=True, stop=False)
                ins = e.matmul(pG.t[:, qt * 8:qt * 8 + 8], lhsT=q_.t[:, qt * 128:(qt + 1) * 128], rhs=km_.t[:, 8:16],
                               start=False, stop=True)
            return ins
        sch.op("pe", gm, reads=[q_.k, km_.k], writes=[pG.k])
        sch.op("dve", lambda e: e.tensor_tensor(out=g64.t[:], in0=pG.t[:, 0:64], in1=cv("pastb64"), op=ALU.add),
               reads=[pG.k, cst.k], writes=[g64.k])

        def mx(e):
            ins = None
            for qt in range(8):
                ins = e.max(out=top64.t[:, qt * 8:qt * 8 + 8], in_=g64.t[:, qt * 8:qt * 8 + 8])
            return ins
        sch.op("dve", mx, reads=[g64.k], writes=[top64.k])

        def ge(e):
            ins = None
            for qt in range(8):
                ins = e.tensor_scalar(out=sel64.t[:, qt * 8:qt * 8 + 8], in0=g64.t[:, qt * 8:qt * 8 + 8],
                                      scalar1=top64.t[:, qt * 8 + 2:qt * 8 + 3], scalar2=None, op0=ALU.is_ge)
            return ins
        sch.op("dve", ge, reads=[g64.k, top64.k], writes=[sel64.k])
        sch.op("dve", lambda e: e.tensor_tensor(out=sel64.t[:], in0=sel64.t[:], in1=cv("past0164"), op=ALU.mult),
               reads=[sel64.k, cst.k], writes=[sel64.k])
        sch.op("dve", lambda e: e.tensor_scalar(out=selq.t[:], in0=sel64.t[:], scalar1=1.0, scalar2=-NEGB,
                                                op0=ALU.subtract, op1=ALU.mult), reads=[sel64.k], writes=[selq.k])

    def a_part2(h):
        st_ = selT[h % 2]

        def tr(e):
            ins = None
            for qt in range(8):
                ins = e.transpose(out=pTr.t[0:8, qt * 128:(qt + 1) * 128], in_=selq.t[:, qt * 8:qt * 8 + 8],
                                  identity=identb.t[:])
            return ins
        sch.op("pe", tr, reads=[selq.k, identb.k], writes=[pTr.k])
        sch.op("dve", lambda e: e.tensor_copy(out=st_.t[0:8, :], in_=pTr.t[0:8, :]), reads=[pTr.k], writes=[st_.k])

    def a_tiles(h, hook):
        q_, k_, v_, s_, st_ = qa[h % 2], ka[h % 2], vA[h % 2], sz[h % 2], selT[h % 2]
        tiles = []
        for qb in range(4):
            nq = 4 + qb
            nkt = 2 * nq + 2
            po_, pr_ = pO[c_["o"] % 2], pR[c_["o"] % 2]
            c_["o"] += 1
            for kt in range(nkt):
                tiles.append((qb, nq, nkt, kt, po_, pr_))

        def emit_qk(t):
            qb, nq, nkt, kt, po_, pr_ = tiles[t]
            nb = kt // 2
            qs = slice(qb * 256, (qb + 1) * 256)
            ps = pS[t % 2]

            def sm(e):
                e.matmul(ps.t[:, 0:256], lhsT=k_.t[:, kt * 128:(kt + 1) * 128], rhs=q_.t[:, qs], start=True, stop=False)
                if nb < nq:
                    return e.matmul(ps.t[:, 0:256], lhsT=Enb.t[:, nb * 128:(nb + 1) * 128], rhs=st_.t[:, qs],
                                    start=False, stop=True)
                cm_ = cmA0 if kt - 2 * nq == 0 else cmA1
                return e.matmul(ps.t[:, 0:256], lhsT=identb.t[:], rhs=cm_.t[:], start=False, stop=True)
            sch.op("pe", sm, reads=[q_.k, k_.k, Enb.k, st_.k, identb.k, cmA0.k, cmA1.k], writes=[ps.k])

        emit_qk(0)
        for t in range(len(tiles)):
            qb, nq, nkt, kt, po_, pr_ = tiles[t]
            qs = slice(qb * 256, (qb + 1) * 256)
            if t + 1 < len(tiles):
                emit_qk(t + 1)
            ps = pS[t % 2]
            p_ = pt[c_["pt"] % 3]
            c_["pt"] += 1
            bo = coff_biasA(cfg, h, qb, kt)
            sch.op("act", lambda e, ps=ps, p_=p_, bo=bo: e.activation(out=p_.t[:, 0:256], in_=ps.t[:, 0:256], func=AF.Exp,
                                                                      bias=cv("biasA", bo, bo + 1), scale=scaleA),
                   reads=[ps.k, cst.k], writes=[p_.k])

            def pv(e, p_=p_, kt=kt, po_=po_, pr_=pr_, nkt=nkt):
                e.matmul(po_.t[:, 0:256], lhsT=v_.t[:, kt, :], rhs=p_.t[:, 0:256], start=(kt == 0), stop=(kt == nkt - 1))
                return e.matmul(pr_.t[:, 0:256], lhsT=onesb.t[:], rhs=p_.t[:, 0:256], start=(kt == 0),
                                stop=(kt == nkt - 1))
            sch.op("pe", pv, reads=[p_.k, v_.k, onesb.k], writes=[po_.k, pr_.k])
            if kt == nkt - 1:
                ri, tt = rinv[qb % 2], t1[qb % 2]
                sch.op("dve", lambda e, ri=ri, pr_=pr_: e.reciprocal(out=ri.t[:, 0:256], in_=pr_.t[:, 0:256]),
                       reads=[pr_.k], writes=[ri.k])
                sch.op("dve", lambda e, ri=ri, tt=tt, po_=po_: e.tensor_tensor(out=tt.t[:, 0:256], in0=po_.t[:, 0:256],
                                                                              in1=ri.t[:, 0:256], op=ALU.mult),
                       reads=[po_.k, ri.k], writes=[tt.k])
                sch.op("dve", lambda e, tt=tt, qs=qs: e.tensor_tensor(out=yT.t[:, h, qs], in0=tt.t[:, 0:256],
                                                                     in1=s_.t[:, qs], op=ALU.mult),
                       reads=[tt.k, s_.k], writes=[yT.k])
                hook(qb)

    a_part1(0)
    a_part1b(0)
    a_part2(0)
    for h in range(cfg.AH):
        def hook(qb, h=h):
            if h + 1 < cfg.AH:
                if qb == 0:
                    a_part1b(h + 1)
                elif qb == 2:
                    a_part2(h + 1)
        if h + 1 < cfg.AH:
            a_part1(h + 1)
        a_tiles(h, hook)
    sch.barrier()
    esb.close()

    esb = ExitStack()
    qB = [alloc(esb, f"b_q{i}", [128, 2, T], BF16) for i in range(2)]
    kB = [alloc(esb, f"b_k{i}", [128, 2, 2048], BF16) for i in range(1)]
    vB = [alloc(esb, f"b_v{i}", [128, 16, 256], BF16) for i in range(1)]
    zB = [alloc(esb, f"b_z{i}", [128, 2, T], BF16) for i in range(2)]
    szB = [alloc(esb, f"b_sz{i}", [128, 2, T], BF16) for i in range(2)]
    ones32 = alloc(esb, "b_ones32", [128, 128], F32)
    dd = [alloc(esb, f"b_dd{i}", [128, 128], F32) for i in range(2)]
    sq = [alloc(esb, f"b_sq{i}", [128, 128], F32) for i in range(2)]
    rs = alloc(esb, "b_rs", [128, 128], F32)
    sch.op("dve", lambda e: e.tensor_copy(out=ones32.t[:], in_=cv("ones")), reads=[cst.k], writes=[ones32.k])
    for h in range(cfg.BH):
        q_, k_, v_, z_, s_ = qB[h % 2], kB[0], vB[0], zB[h % 2], szB[h % 2]
        r0 = h * 256
        sch.dma("sp", q_.t[:], pT[d["PQB"] + r0:d["PQB"] + r0 + 256, :].rearrange("(m p) t -> p m t", p=128), q_,
                reads=RD, writes=[q_.k])
        sch.dma("sp", k_.t[:, :, 0:T], d["kva"][1][r0:r0 + 256, :].rearrange("(m p) t -> p m t", p=128), k_,
                reads=RKA, writes=[k_.k])
        sch.dma("sp", k_.t[:, :, T:2 * T], d["kvo"][1][r0:r0 + 256, :].rearrange("(m p) t -> p m t", p=128), k_,
                reads=RKV, multi=[k_.k])
        with ncd():
            sch.dma("sp", v_.t[:, 0:8, :], d["kva"][3][0:T, r0:r0 + 256].rearrange("(kt p) c -> p kt c", p=128), v_,
                    reads=RKA, writes=[v_.k])
            sch.dma("sp", v_.t[:, 8:16, :], d["kvo"][3][:, r0:r0 + 256].rearrange("(kt p) c -> p kt c", p=128), v_,
                    reads=RKV, multi=[v_.k])
        sch.dma("sp", z_.t[:], pT[d["PZB"] + r0:d["PZB"] + r0 + 256, :].rearrange("(m p) t -> p m t", p=128), z_,
                reads=RD, writes=[z_.k])
        sch.op("act", lambda e, z_=z_, s_=s_: e.activation(out=s_.t[:], in_=z_.t[:], func=AF.Silu),
               reads=[z_.k], writes=[s_.k])
        tiles = []
        for qt in range(8):
            nkt = 8 + qt + 1
            po_, pr_ = pO[c_["o"] % 2], pR[c_["o"] % 2]
            c_["o"] += 1
            for kt in range(nkt):
                tiles.append((qt, nkt, kt, po_, pr_))

        def emit_qk(t, q_=q_, k_=k_, tiles=tiles):
            qt, nkt, kt, po_, pr_ = tiles[t]
            qs = slice(qt * 128, (qt + 1) * 128)
            ps = pS[t % 2]
            diag = (kt == nkt - 1)

            def sm(e):
                ins = None
                for m in range(2):
                    ins = e.matmul(ps.t[:, m * 128:(m + 1) * 128], lhsT=k_.t[:, m, kt * 128:(kt + 1) * 128],
                                   rhs=q_.t[:, m, qs], start=True, stop=not diag)
                    if diag:
                        ins = e.matmul(ps.t[:, m * 128:(m + 1) * 128], lhsT=identb.t[:], rhs=cmB.t[:, 0:128],
                                       start=False, stop=True)
                return ins
            sch.op("pe", sm, reads=[q_.k, k_.k, identb.k, cmB.k], writes=[ps.k])

        def ep1(qt, po_, pr_):
            ri, tt = rinv[qt % 2], t1[qt % 2]
            sch.op("dve", lambda e, ri=ri, pr_=pr_: e.reciprocal(out=ri.t[:, 0:256], in_=pr_.t[:, 0:256]),
                   reads=[pr_.k], writes=[ri.k])
            for c2 in range(2):
                sch.op("dve", lambda e, ri=ri, tt=tt, po_=po_, c2=c2: e.tensor_tensor(
                    out=tt.t[:, c2 * 256:(c2 + 1) * 256], in0=po_.t[:, c2 * 256:(c2 + 1) * 256], in1=ri.t[:, 0:256],
                    op=ALU.mult), reads=[po_.k, ri.k], writes=[tt.k])
                sch.op("dve", lambda e, tt=tt, c2=c2: e.scalar_tensor_tensor(
                    out=dd[c2].t[:], in0=tt.t[:, c2 * 256 + 128:c2 * 256 + 256], scalar=neglam,
                    in1=tt.t[:, c2 * 256:c2 * 256 + 128], op0=ALU.mult, op1=ALU.add),
                    reads=[tt.k, lsc.k], writes=[dd[c2].k])
                sch.op("dve", lambda e, c2=c2: e.tensor_tensor(out=sq[c2].t[:], in0=dd[c2].t[:], in1=dd[c2].t[:], op=ALU.mult),
                       reads=[dd[c2].k], writes=[sq[c2].k])

        def ep2(qt, s_=s_, h=h):
            qs = slice(qt * 128, (qt + 1) * 128)

            def msm(e):
                e.matmul(pG.t[:, 0:128], lhsT=ones32.t[:], rhs=sq[0].t[:], start=True, stop=False)
                return e.matmul(pG.t[:, 0:128], lhsT=ones32.t[:], rhs=sq[1].t[:], start=False, stop=True)
            sch.op("pe", msm, reads=[ones32.k, sq[0].k, sq[1].k], writes=[pG.k])
            sch.op("dve", lambda e: e.tensor_scalar(out=rs.t[:], in0=pG.t[:, 0:128], scalar1=1.0 / 256, scalar2=RMS_EPS,
                                                    op0=ALU.mult, op1=ALU.add), reads=[pG.k], writes=[rs.k])

        def ep2b(qt, s_=s_, h=h):
            qs = slice(qt * 128, (qt + 1) * 128)
            sch.op("act", lambda e: e.activation(out=rs.t[:], in_=rs.t[:], func=AF.Ln), reads=[rs.k], writes=[rs.k])
            sch.op("act", lambda e: e.activation(out=rs.t[:], in_=rs.t[:], func=AF.Exp, scale=-0.5), reads=[rs.k], writes=[rs.k])
            for c2 in range(2):
                sch.op("dve", lambda e, c2=c2: e.scalar_tensor_tensor(out=dd[c2].t[:], in0=dd[c2].t[:], scalar=subg.t[:, c2:c2 + 1],
                                                                     in1=rs.t[:], op0=ALU.mult, op1=ALU.mult),
                       reads=[dd[c2].k, subg.k, rs.k], writes=[dd[c2].k])
                sch.op("dve", lambda e, c2=c2, s_=s_, qs=qs, h=h: e.tensor_tensor(
                    out=yT.t[:, BC + h * 2 + c2, qs], in0=dd[c2].t[:], in1=s_.t[:, c2, qs], op=ALU.mult),
                    reads=[dd[c2].k, s_.k], writes=[yT.k])

        emit_qk(0)
        pend = None
        for t in range(len(tiles)):
            qt, nkt, kt, po_, pr_ = tiles[t]
            if t + 1 < len(tiles):
                emit_qk(t + 1)
            if pend is not None and kt == 7:
                ep2(pend)
            if pend is not None and kt == 9:
                ep2b(pend)
                pend = None
            ps = pS[t % 2]
            p_ = pt[c_["pt"] % 3]
            c_["pt"] += 1
            bo = (h * 8 + qt) * 16 + kt
            sch.op("act", lambda e, ps=ps, p_=p_, bo=bo: e.activation(out=p_.t[:, 0:256], in_=ps.t[:, 0:256], func=AF.Exp,
                                                                      bias=cv("biasB", bo, bo + 1), scale=scaleA),
                   reads=[ps.k, cst.k], writes=[p_.k])

            def pv(e, p_=p_, kt=kt, v_=v_, po_=po_, pr_=pr_, nkt=nkt):
                for c2 in range(2):
                    e.matmul(po_.t[:, c2 * 256:(c2 + 1) * 256], lhsT=v_.t[:, kt, c2 * 128:(c2 + 1) * 128],
                             rhs=p_.t[:, 0:256], start=(kt == 0), stop=(kt == nkt - 1))
                return e.matmul(pr_.t[:, 0:256], lhsT=onesb.t[:], rhs=p_.t[:, 0:256], start=(kt == 0),
                                stop=(kt == nkt - 1))
            sch.op("pe", pv, reads=[p_.k, v_.k, onesb.k], writes=[po_.k, pr_.k])
            if kt == nkt - 1:
                ep1(qt, po_, pr_)
                pend = qt
        ep2(pend)
        ep2b(pend)
    sch.barrier()
    esb.close()
    esb = ExitStack()
    scaleM = 256.0 ** -0.5
    qM = [alloc(esb, f"mm_q{i}", [128, 2, T], BF16) for i in range(2)]
    zM = [alloc(esb, f"mm_z{i}", [128, 2, T], BF16) for i in range(2)]
    szM = [alloc(esb, f"mm_sz{i}", [128, 2, T], BF16) for i in range(2)]
    kM = [alloc(esb, f"mm_k{i}", [128, 2, 256], BF16) for i in range(2)]
    vM = [alloc(esb, f"mm_v{i}", [128, 2, 256], BF16) for i in range(2)]
    for h in range(cfg.MH):
        q_, z_, s_, k_, v_ = qM[h % 2], zM[h % 2], szM[h % 2], kM[h % 2], vM[h % 2]
        r0 = h * 256
        sch.dma("sp", q_.t[:], pT[d["PQM"] + r0:d["PQM"] + r0 + 256, :].rearrange("(m p) t -> p m t", p=128), q_,
                reads=RD, writes=[q_.k])
        sch.dma("sp", z_.t[:], pT[d["PZM"] + r0:d["PZM"] + r0 + 256, :].rearrange("(m p) t -> p m t", p=128), z_,
                reads=RD, writes=[z_.k])
        sch.dma("sp", k_.t[:], d["mkT"][r0:r0 + 256, :].rearrange("(m p) t -> p m t", p=128), k_, reads=RD, writes=[k_.k])
        with ncd():
            sch.dma("sp", v_.t[:], d["mv"][:, r0:r0 + 256].rearrange("(mt p) c -> p mt c", p=128), v_, reads=RD,
                    writes=[v_.k])
        sch.op("act", lambda e, z_=z_, s_=s_: e.activation(out=s_.t[:], in_=z_.t[:], func=AF.Silu),
               reads=[z_.k], writes=[s_.k])
        for hh in range(2):
            qs = slice(hh * 512, (hh + 1) * 512)
            pr_ = pR[hh % 2]
            for mt in range(2):
                ps = pS[c_["s"] % 2]
                c_["s"] += 1
                p_ = pt[c_["pt"] % 3]
                c_["pt"] += 1

                def sm(e, ps=ps, mt=mt, q_=q_, k_=k_, qs=qs):
                    e.matmul(ps.t[:, :], lhsT=k_.t[:, 0, mt * 128:(mt + 1) * 128], rhs=q_.t[:, 0, qs], start=True, stop=False)
                    return e.matmul(ps.t[:, :], lhsT=k_.t[:, 1, mt * 128:(mt + 1) * 128], rhs=q_.t[:, 1, qs], start=False,
                                    stop=True)
                sch.op("pe", sm, reads=[q_.k, k_.k], writes=[ps.k])
                sch.op("act", lambda e, ps=ps, p_=p_: e.activation(out=p_.t[:, :], in_=ps.t[:, :], func=AF.Exp, scale=scaleM),
                       reads=[ps.k], writes=[p_.k])

                def pv(e, p_=p_, mt=mt, v_=v_, pr_=pr_):
                    for c2 in range(2):
                        e.matmul(pO[c2].t[:, :], lhsT=v_.t[:, mt, c2 * 128:(c2 + 1) * 128], rhs=p_.t[:, :], start=(mt == 0),
                                 stop=(mt == 1))
                    return e.matmul(pr_.t[:, :], lhsT=onesb.t[:], rhs=p_.t[:, :], start=(mt == 0), stop=(mt == 1))
                sch.op("pe", pv, reads=[p_.k, v_.k, onesb.k], writes=[pO[0].k, pO[1].k, pr_.k])
            ri = rinv[hh % 2]
            sch.op("dve", lambda e, ri=ri, pr_=pr_: e.reciprocal(out=ri.t[:, :], in_=pr_.t[:, :]), reads=[pr_.k], writes=[ri.k])
            for c2 in range(2):
                tt = t1[c2]
                sch.op("dve", lambda e, ri=ri, tt=tt, c2=c2: e.tensor_tensor(out=tt.t[:, :], in0=pO[c2].t[:, :], in1=ri.t[:, :],
                                                                            op=ALU.mult), reads=[pO[c2].k, ri.k], writes=[tt.k])
                sch.op("dve", lambda e, tt=tt, c2=c2, s_=s_, qs=qs, h=h: e.tensor_tensor(
                    out=yT.t[:, 3 * BC + h * 2 + c2, qs], in0=tt.t[:, :], in1=s_.t[:, c2, qs], op=ALU.mult),
                    reads=[tt.k, s_.k], writes=[yT.k])
    sch.barrier()
    esb.close()
    esb = ExitStack()
    SG = cfg.SG
    GC = 1.5957691216057308
    wT32 = alloc(esb, "c_wT32", [128, SG * 128], F32)
    wTb = alloc(esb, "c_wTb", [128, SG * 128], BF16)
    bsb = alloc(esb, "c_bsb", [128, SG * 128], F32)
    lg = alloc(esb, "c_lg", [128, BW], F32)
    lb = alloc(esb, "c_lb", [128, BW], F32)
    sch.dma("sp", wT32.t[:], d["sguw"][:, :], wT32, writes=[wT32.k])
    sch.dma("sp", bsb.t[:], d["sgubs"][:, :], bsb, writes=[bsb.k])
    sch.dma("sp", lg.t[:], d["sgug"][:, :], lg, writes=[lg.k])
    sch.dma("sp", lb.t[:], d["sgub"][:, :], lb, writes=[lb.k])
    for g in range(SG):
        sch.op("dve", lambda e, g=g: e.tensor_tensor(out=wTb.t[:, g * 128:(g + 1) * 128], in0=wT32.t[:, g * 128:(g + 1) * 128],
                                                     in1=cv("tri"), op=ALU.mult), reads=[wT32.k, cst.k], writes=[wTb.k])
    vcb = [alloc(esb, f"c_vcb{i}", [128, BW], BF16) for i in range(2)]
    ucb = [alloc(esb, f"c_ucb{i}", [128, BW], BF16) for i in range(2)]
    zcb = [alloc(esb, f"c_zcb{i}", [128, BW], BF16) for i in range(2)]
    fa = alloc(esb, "c_fa", [128, BW], F32)
    fb = alloc(esb, "c_fb", [128, BW], F32)
    fc = alloc(esb, "c_fc", [128, BW], F32)
    vn = alloc(esb, "c_vn", [128, BW], BF16)
    cs = alloc(esb, "c_cs", [128, 8], F32)

    def gelu(src, dst, tmp):
        sch.op("dve", lambda e: e.tensor_tensor(out=tmp.t[:], in0=src.t[:], in1=src.t[:], op=ALU.mult),
               reads=[src.k], writes=[tmp.k])
        sch.op("dve", lambda e: e.tensor_scalar(out=tmp.t[:], in0=tmp.t[:], scalar1=0.044715, scalar2=1.0, op0=ALU.mult,
                                                op1=ALU.add), reads=[tmp.k], writes=[tmp.k])
        sch.op("dve", lambda e: e.tensor_tensor(out=tmp.t[:], in0=tmp.t[:], in1=src.t[:], op=ALU.mult),
               reads=[src.k, tmp.k], writes=[tmp.k])
        sch.op("act", lambda e: e.activation(out=tmp.t[:], in_=tmp.t[:], func=AF.Sigmoid, scale=GC),
               reads=[tmp.k], writes=[tmp.k])
        sch.op("dve", lambda e: e.tensor_tensor(out=dst.t[:], in0=tmp.t[:], in1=src.t[:], op=ALU.mult),
               reads=[src.k, tmp.k], writes=[dst.k])

    for ci in range(8):
        v_, u_, z_ = vcb[ci % 2], ucb[ci % 2], zcb[ci % 2]
        ts = slice(ci * 128, (ci + 1) * 128)
        sch.dma("sp", v_.t[:], d["vc"][ci * 128:(ci + 1) * 128, :], v_, reads=RD, writes=[v_.k])
        with ncd():
            sch.dma("sp", u_.t[:].rearrange("p (g t) -> p g t", t=128),
                    pT[d["PUC"]:d["PUC"] + BW, ts].rearrange("(g p) t -> p g t", p=128), u_, reads=RD, writes=[u_.k])
            sch.dma("sp", z_.t[:].rearrange("p (g t) -> p g t", t=128),
                    pT[d["PZC"]:d["PZC"] + BW, ts].rearrange("(g p) t -> p g t", p=128), z_, reads=RD, writes=[z_.k])
        gelu(v_, fa, fb)
        sch.op("dve", lambda e: e.reduce_sum(out=cs.t[:, 0:1], in_=fa.t[:], axis=AX.X), reads=[fa.k], writes=[cs.k])
        sch.op("dve", lambda e: e.tensor_scalar(out=cs.t[:, 1:2], in0=cs.t[:, 0:1], scalar1=-1.0 / BW, scalar2=None,
                                                op0=ALU.mult), reads=[cs.k], writes=[cs.k])
        sch.op("dve", lambda e: e.tensor_scalar(out=fa.t[:], in0=fa.t[:], scalar1=cs.t[:, 1:2], scalar2=None, op0=ALU.add),
               reads=[fa.k, cs.k], writes=[fa.k])
        sch.op("act", lambda e: e.activation(out=vn.t[:], in_=fa.t[:], func=AF.Square, accum_out=cs.t[:, 2:3]),
               reads=[fa.k], writes=[vn.k, cs.k])
        sch.op("dve", lambda e: e.tensor_scalar(out=cs.t[:, 3:4], in0=cs.t[:, 2:3], scalar1=1.0 / BW, scalar2=LN_EPS,
                                                op0=ALU.mult, op1=ALU.add), reads=[cs.k], writes=[cs.k])
        sch.op("act", lambda e: e.activation(out=cs.t[:, 4:5], in_=cs.t[:, 3:4], func=AF.Sqrt), reads=[cs.k], writes=[cs.k])
        sch.op("dve", lambda e: e.reciprocal(out=cs.t[:, 5:6], in_=cs.t[:, 4:5]), reads=[cs.k], writes=[cs.k])
        sch.op("dve", lambda e: e.scalar_tensor_tensor(out=fa.t[:], in0=fa.t[:], scalar=cs.t[:, 5:6], in1=lg.t[:],
                                                       op0=ALU.mult, op1=ALU.mult), reads=[fa.k, cs.k, lg.k], writes=[fa.k])
        sch.op("dve", lambda e: e.tensor_tensor(out=vn.t[:], in0=fa.t[:], in1=lb.t[:], op=ALU.add),
               reads=[fa.k, lb.k], writes=[vn.k])
        pm = [pO[0], pO[1]]

        def mix(e):
            ins = None
            for g in range(SG):
                ins = e.matmul(pm[g // 4].t[:, (g % 4) * 128:(g % 4 + 1) * 128], lhsT=vn.t[:, g * 128:(g + 1) * 128],
                               rhs=wTb.t[:, g * 128:(g + 1) * 128], start=True, stop=True)
            return ins
        sch.op("pe", mix, reads=[vn.k, wTb.k], writes=[pO[0].k, pO[1].k])
        for g4 in range((SG + 3) // 4):
            n = min(4, SG - g4 * 4) * 128
            sch.op("dve", lambda e, g4=g4, n=n: e.tensor_tensor(out=fc.t[:, g4 * 512:g4 * 512 + n], in0=pm[g4].t[:, 0:n],
                                                              in1=bsb.t[:, g4 * 512:g4 * 512 + n], op=ALU.add),
                   reads=[pm[g4].k, bsb.k], writes=[fc.k])
        gelu(u_, fa, fb)
        sch.op("dve", lambda e: e.tensor_tensor(out=fc.t[:], in0=fc.t[:], in1=fa.t[:], op=ALU.mult), reads=[fc.k, fa.k],
               writes=[fc.k])
        sch.op("act", lambda e, z_=z_: e.activation(out=fb.t[:], in_=z_.t[:], func=AF.Silu), reads=[z_.k], writes=[fb.k])
        sch.op("dve", lambda e, ts=ts: e.tensor_tensor(out=yT.t[:, 2 * BC:3 * BC, ts],
                                                       in0=fc.t[:].rearrange("p (g t) -> p g t", t=128),
                                                       in1=fb.t[:].rearrange("p (g t) -> p g t", t=128), op=ALU.mult),
               reads=[fc.k, fb.k], writes=[yT.k])
    sch.barrier()
    esb.close()


def coff_biasA(cfg, h, qb, kt):
    return (h * 4 + qb) * 16 + kt


_NC_CACHE = {}


def _get_nc(cfg, layer_ids):
    key = (cfg.D, cfg.ncores, tuple(layer_ids))
    if key not in _NC_CACHE:
        _NC_CACHE[key] = build_program(cfg, list(layer_ids))
    return _NC_CACHE[key]


def _bc(v, n=128):
    v = np.asarray(v, np.float32).reshape(1, -1)
    return np.ascontiguousarray(np.broadcast_to(v, (n, v.shape[1])))


def run_layers(cfg, layer_ids, x_full, inp):
    T, D, BW = cfg.T, cfg.D, cfg.BW
    nc = _get_nc(cfg, layer_ids)
    f = lambda a: np.ascontiguousarray(np.asarray(a, np.float32))
    shared = {"fgvec": _bc(inp["final_g"])}
    for l in layer_ids:
        s = f"_{l}"
        shared.update({
            "w_in" + s: f(inp["w_in"][l]),
            "w_mkv" + s: f(inp["w_mem_kv"][l]),
            "w_br" + s: f(np.asarray(inp["w_branch"][l]).reshape(4 * BW, D)),
            "w_out" + s: f(inp["w_out"][l]),
            "gvec" + s: _bc(inp["norm_g"][l]),
            "mgvec" + s: _bc(inp["mem_norm_g"][l]),
            "lamv" + s: np.ascontiguousarray(np.concatenate(
                [_bc(inp[k][l]) for k in ("diff_lam_q1", "diff_lam_k1", "diff_lam_q2", "diff_lam_k2")], 1)),
            "subg" + s: f(np.asarray(inp["diff_subln_g"][l]).reshape(2, 128).T),
            "sgug" + s: _bc(inp["sgu_ln_g"][l]),
            "sgub" + s: _bc(inp["sgu_ln_b"][l]),
            "sguwT" + s: f(np.transpose(np.asarray(inp["sgu_w"][l]), (2, 0, 1)).reshape(128, cfg.SG * 128)),
            "sgubs" + s: _bc(np.asarray(inp["sgu_b"][l]).reshape(-1)),
        })
    csts = [build_consts(cfg, 0), build_consts(cfg, 1)]
    in_maps = []
    for c in range(cfg.ncores):
        b, hf = c // 2, c % 2
        m = dict(shared)
        m["xo"] = f(x_full[b, hf * T:(hf + 1) * T])
        m["mem"] = f(inp["mem"][b])
        m["cst"] = csts[hf]
        in_maps.append(m)
    res = run_bass_kernel_spmd(nc, in_maps, core_ids=list(range(cfg.ncores)))
    out = np.empty_like(np.asarray(x_full, np.float32))
    for c in range(cfg.ncores):
        b, hf = c // 2, c % 2
        out[b, hf * T:(hf + 1) * T] = res.results[c]["out"]
    return out


def kernel(**inputs):
    cfg = Cfg(D=4096, depth=2)
    x = np.asarray(inputs["x"], np.float32)
    return run_layers(cfg, list(range(cfg.depth)), x, inputs)
```
